# Optimizing a Trainium2 kernel written in Bass

```python
import math
import jax, jax.numpy as jnp
from jax import lax
import numpy as np

D_MODEL = 2048
BATCH = 8
SEQ = 2048
DEPTH = 4

D_MIX = D_MODEL
MLA_HEADS = 8
MLA_NOPE = 128
MLA_ROPE = 64
MLA_V = 128
KV_RANK = 512
RET_HEADS = 4
RET_DK = 128
RET_DV = 128
HG_HEADS = 4
HG_DK = 128
HG_DV = 128
D_FF = 5632
Q_BLOCK = 128
RET_CHUNK = 128
HG_CHUNK = 64
ROPE_BASE = 10000.0
EPS = 1e-6
MASK_VALUE = -1e30
MIN_FORGET = 1e-20

MLA_OUT = MLA_HEADS * MLA_V
RET_OUT = RET_HEADS * RET_DV
HG_OUT = HG_HEADS * HG_DV
IN_SPLITS = (MLA_HEADS * (MLA_NOPE + MLA_ROPE), KV_RANK, MLA_ROPE,
             RET_HEADS * RET_DK, RET_HEADS * RET_DK, RET_OUT, RET_OUT,
             HG_HEADS * HG_DK, HG_HEADS * HG_DK, HG_OUT, HG_OUT)
D_IN = sum(IN_SPLITS)

kernel_name = "hymba_mla_retnet_hgrn2_macaron"


def rms_norm(x, w):
    xf = x.astype(jnp.float32)
    y = xf * lax.rsqrt(jnp.mean(xf * xf, axis=-1, keepdims=True) + EPS)
    return y.astype(x.dtype) * w


def rope_tables(n, dim):
    inv = ROPE_BASE ** (-jnp.arange(0, dim, 2, dtype=jnp.float32) / dim)
    ang = jnp.arange(n, dtype=jnp.float32)[:, None] * inv[None, :]
    return jnp.cos(ang), jnp.sin(ang)


def apply_rope(x, cos, sin):
    shape = (1, cos.shape[0]) + (1,) * (x.ndim - 3) + (cos.shape[1],)
    c = cos.reshape(shape).astype(x.dtype)
    s = sin.reshape(shape).astype(x.dtype)
    x1, x2 = jnp.split(x, 2, axis=-1)
    return jnp.concatenate([x1 * c - x2 * s, x2 * c + x1 * s], axis=-1)


def swiglu(x, w1, w3, w2):
    return (jax.nn.silu(x @ w1) * (x @ w3)) @ w2


def mla_attention(q_all, c_kv, k_rope, kv_norm_w, w_kv_b, out_norm_w, cos, sin):
    B, S, _ = q_all.shape
    q = q_all.reshape(B, S, MLA_HEADS, MLA_NOPE + MLA_ROPE)
    q_nope = q[..., :MLA_NOPE]
    q_rope = apply_rope(q[..., MLA_NOPE:], cos, sin)
    k_rope = apply_rope(k_rope, cos, sin)
    kv = (rms_norm(c_kv, kv_norm_w) @ w_kv_b).reshape(B, S, MLA_HEADS, MLA_NOPE + MLA_V)
    k_nope, v = kv[..., :MLA_NOPE], kv[..., MLA_NOPE:]
    scale = (MLA_NOPE + MLA_ROPE) ** -0.5
    outs = []
    for blk in range(S // Q_BLOCK):
        q0, q1 = blk * Q_BLOCK, (blk + 1) * Q_BLOCK
        s = (jnp.einsum('bqhd,bkhd->bhqk', q_nope[:, q0:q1], k_nope[:, :q1])
             + jnp.einsum('bqhr,bkr->bhqk', q_rope[:, q0:q1], k_rope[:, :q1]))
        s = s.astype(jnp.float32) * scale
        qpos = jnp.arange(q0, q1)
        kpos = jnp.arange(q1)
        s = jnp.where(kpos[None, :] <= qpos[:, None], s, MASK_VALUE)
        p = jax.nn.softmax(s, axis=-1).astype(v.dtype)
        outs.append(jnp.einsum('bhqk,bkhd->bqhd', p, v[:, :q1]))
    o = jnp.concatenate(outs, axis=1)
    o = rms_norm(o, out_norm_w.reshape(MLA_HEADS, MLA_V))
    return o.reshape(B, S, MLA_OUT)


def retention(q, k, v, g, gn_w, cos, sin):
    B, S, _ = q.shape
    dt = g.dtype
    q = apply_rope(q.reshape(B, S, RET_HEADS, RET_DK), cos, sin)
    k = apply_rope(k.reshape(B, S, RET_HEADS, RET_DK), cos, sin) * (RET_DK ** -0.5)
    v = v.reshape(B, S, RET_HEADS, RET_DV)
    log_gamma = jnp.log1p(-(2.0 ** (-5.0 - jnp.arange(RET_HEADS, dtype=jnp.float32))))
    idx = jnp.arange(RET_CHUNK, dtype=jnp.float32)
    rel = idx[:, None] - idx[None, :]
    decay = jnp.where(rel >= 0, jnp.exp(log_gamma[:, None, None] * jnp.maximum(rel, 0.0)), 0.0)
    xi = jnp.exp(log_gamma[None, :] * (idx[:, None] + 1.0))
    zeta = jnp.exp(log_gamma[:, None] * (RET_CHUNK - 1.0 - idx[None, :]))
    g_chunk = jnp.exp(log_gamma * RET_CHUNK)
    n = S // RET_CHUNK

    def to_chunks(t):
        return t.astype(jnp.float32).reshape(B, n, RET_CHUNK, RET_HEADS, -1).swapaxes(0, 1)

    def step(R, inp):
        qi, ki, vi = inp
        intra = jnp.einsum('bihd,bjhd->bhij', qi, ki) * decay[None]
        o = (jnp.einsum('bhij,bjhe->bihe', intra, vi)
             + jnp.einsum('bihd,bhde->bihe', qi, R) * xi[None, :, :, None])
        R = R * g_chunk[None, :, None, None] + jnp.einsum('bjhd,bjhe,hj->bhde', ki, vi, zeta)
        return R, o

    R0 = jnp.zeros((B, RET_HEADS, RET_DK, RET_DV), jnp.float32)
    _, o = lax.scan(step, R0, (to_chunks(q), to_chunks(k), to_chunks(v)))
    o = o.swapaxes(0, 1).reshape(B, S, RET_HEADS, RET_DV)
    mu = jnp.mean(o, axis=-1, keepdims=True)
    var = jnp.mean(jnp.square(o - mu), axis=-1, keepdims=True)
    o = ((o - mu) * lax.rsqrt(var + EPS)).astype(dt) * gn_w.reshape(RET_HEADS, RET_DV)
    return jax.nn.silu(g) * o.reshape(B, S, RET_OUT)


def hgrn2(q, f_logit, i_in, g, lb, norm_w):
    B, S, _ = q.shape
    dt = g.dtype
    shp = (B, S, HG_HEADS, HG_DK)
    q = q.reshape(shp).astype(jnp.float32)
    z = f_logit.reshape(shp).astype(jnp.float32)
    v = i_in.reshape(B, S, HG_HEADS, HG_DV).astype(jnp.float32)
    lb = lb.reshape(HG_HEADS, HG_DK).astype(jnp.float32)
    f = lb + (1.0 - lb) * jax.nn.sigmoid(z)
    log_f = jnp.log(jnp.maximum(f, MIN_FORGET))
    k = (1.0 - lb) * jax.nn.sigmoid(-z)
    n = S // HG_CHUNK
    causal = (jnp.arange(HG_CHUNK)[:, None] >= jnp.arange(HG_CHUNK)[None, :])[None, :, :, None, None]

    def to_chunks(t):
        return t.reshape(B, n, HG_CHUNK, HG_HEADS, -1).swapaxes(0, 1)

    def step(St, inp):
        qi, ki, vi, lfi = inp
        b = jnp.cumsum(lfi, axis=1)
        diff = b[:, :, None] - b[:, None, :]
        dec = jnp.where(causal, jnp.exp(jnp.where(causal, diff, 0.0)), 0.0)
        A = jnp.einsum('bihk,bjhk,bijhk->bhij', qi, ki, dec)
        o = (jnp.einsum('bhij,bjhv->bihv', A, vi)
             + jnp.einsum('bihk,bhkv->bihv', qi * jnp.exp(b), St))
        b_last = b[:, -1:]
        St = (jnp.exp(b_last[:, 0])[..., None] * St
              + jnp.einsum('bjhk,bjhv->bhkv', ki * jnp.exp(b_last - b), vi))
        return St, o

    S0 = jnp.zeros((B, HG_HEADS, HG_DK, HG_DV), jnp.float32)
    _, o = lax.scan(step, S0, (to_chunks(q), to_chunks(k), to_chunks(v), to_chunks(log_f)))
    o = o.swapaxes(0, 1).reshape(B, S, HG_HEADS, HG_DV)
    o = o * lax.rsqrt(jnp.mean(o * o, axis=-1, keepdims=True) + EPS)
    o = o.astype(dt) * norm_w.reshape(HG_HEADS, HG_DV)
    return jax.nn.silu(g) * o.reshape(B, S, HG_OUT)


def token_mixer(h, w_in, kv_norm_w, w_kv_b, mla_norm_w, ret_gn_w, lb, hg_norm_w, w_o, rope_mla, rope_ret):
    proj = h @ w_in
    cuts = [int(c) for c in np.cumsum(IN_SPLITS)[:-1]]
    (mq, ckv, krope, rq, rk, rv, rg, hq, hf, hi, hg) = jnp.split(proj, cuts, axis=-1)
    o_a = mla_attention(mq, ckv, krope, kv_norm_w, w_kv_b, mla_norm_w, *rope_mla)
    o_b = retention(rq, rk, rv, rg, ret_gn_w, *rope_ret)
    o_c = hgrn2(hq, hf, hi, hg, lb, hg_norm_w)
    return jnp.concatenate([o_a, o_b, o_c], axis=-1) @ w_o


def setup_inputs(seed: int = 0) -> dict:
    key = jax.random.key(seed)
    ks = jax.random.split(key, 20)
    nrm = lambda k, shape, fan: jax.random.normal(k, shape, jnp.float32) * (fan ** -0.5)
    gain = lambda k, shape: 1.0 + 0.02 * jax.random.normal(k, shape, jnp.float32)
    D = D_MODEL
    return {
        "x": jax.random.normal(ks[0], (BATCH, SEQ, D), jnp.float32),
        "ffn1_norm": gain(ks[1], (DEPTH, D)),
        "ffn1_w1": nrm(ks[2], (DEPTH, D, D_FF), D),
        "ffn1_w3": nrm(ks[3], (DEPTH, D, D_FF), D),
        "ffn1_w2": nrm(ks[4], (DEPTH, D_FF, D), D_FF),
        "mix_norm": gain(ks[5], (DEPTH, D)),
        "w_in": nrm(ks[6], (DEPTH, D, D_IN), D),
        "mla_kv_norm": gain(ks[7], (DEPTH, KV_RANK)),
        "mla_w_kv_b": nrm(ks[8], (DEPTH, KV_RANK, MLA_HEADS * (MLA_NOPE + MLA_V)), KV_RANK),
        "mla_out_norm": gain(ks[9], (DEPTH, MLA_OUT)),
        "ret_gn": gain(ks[10], (DEPTH, RET_OUT)),
        "hgrn_lb_logits": 0.5 * jax.random.normal(ks[11], (DEPTH, HG_HEADS * HG_DK), jnp.float32),
        "hgrn_out_norm": gain(ks[12], (DEPTH, HG_OUT)),
        "w_o": nrm(ks[13], (DEPTH, D_MIX, D), D_MIX),
        "ffn2_norm": gain(ks[14], (DEPTH, D)),
        "ffn2_w1": nrm(ks[15], (DEPTH, D, D_FF), D),
        "ffn2_w3": nrm(ks[16], (DEPTH, D, D_FF), D),
        "ffn2_w2": nrm(ks[17], (DEPTH, D_FF, D), D_FF),
        "final_norm": gain(ks[18], (D,)),
    }


def reference(x, ffn1_norm, ffn1_w1, ffn1_w3, ffn1_w2, mix_norm, w_in, mla_kv_norm, mla_w_kv_b,
              mla_out_norm, ret_gn, hgrn_lb_logits, hgrn_out_norm, w_o, ffn2_norm, ffn2_w1,
              ffn2_w3, ffn2_w2, final_norm):
    S = x.shape[1]
    rope_mla = rope_tables(S, MLA_ROPE)
    rope_ret = rope_tables(S, RET_DK)
    p = jax.nn.softmax(hgrn_lb_logits.astype(jnp.float32), axis=0)
    lbs = jnp.cumsum(p, axis=0) - p[0:1]
    h = x
    for l in range(DEPTH):
        h = h + 0.5 * swiglu(rms_norm(h, ffn1_norm[l]), ffn1_w1[l], ffn1_w3[l], ffn1_w2[l])
        h = h + token_mixer(rms_norm(h, mix_norm[l]), w_in[l], mla_kv_norm[l], mla_w_kv_b[l],
                            mla_out_norm[l], ret_gn[l], lbs[l], hgrn_out_norm[l], w_o[l],
                            rope_mla, rope_ret)
        h = h + 0.5 * swiglu(rms_norm(h, ffn2_norm[l]), ffn2_w1[l], ffn2_w3[l], ffn2_w2[l])
    return rms_norm(h, final_norm)
```

```python
from contextlib import ExitStack

import numpy as np
import concourse.bass as bass
import concourse.mybir as mybir
from concourse.bass_utils import run_bass_kernel_spmd

F32 = mybir.dt.float32
BF16 = mybir.dt.bfloat16
ALU = mybir.AluOpType
AF = mybir.ActivationFunctionType
AX = mybir.AxisListType

D = 2048
S = 2048
DEPTH = 4
DFF = 5632
NFF = DFF // 128
KC = D // 128
D_IN = 6208
EPS = 1e-6
N_CORES = 8
DEBUG_STOP = 3


class Tok:
    __slots__ = ("sem", "val", "eng")

    def __init__(self, sem, val, eng):
        self.sem, self.val, self.eng = sem, val, eng


class Res:
    __slots__ = ("name", "w", "r")

    def __init__(self, name=""):
        self.name = name
        self.w = None
        self.r = {}


class Sched:
    COMPUTE = ("pe", "dve", "act", "pool")
    ROT = 12000

    def __init__(self, n_dma_sems=24):
        self.streams = {e: [] for e in ("pe", "dve", "act", "pool", "sp")}
        self.n_sems = 0
        self.cur = {}
        for e in self.COMPUTE:
            self.cur[e] = [self._new_sem(), 0]
        self.dsem = {q: [[self._new_sem(), 0] for _ in range(n_dma_sems)] for q in ("sp", "pool", "act")}
        self.dma_rr = {q: 0 for q in self.dsem}
        self.waited = {e: {} for e in self.streams}
        self.pending = {e: [] for e in self.COMPUTE}
        self.n_ops = 0

    def _new_sem(self):
        self.n_sems += 1
        return self.n_sems - 1

    def _deps(self, reads, writes):
        deps = []
        for r in reads:
            if r.w is not None:
                deps.append(r.w)
        for w in writes:
            if w.w is not None:
                deps.append(w.w)
            deps.extend(w.r.values())
        return deps

    def _emit_waits(self, eng, deps):
        st = self.streams[eng]
        wd = self.waited[eng]
        for t in deps:
            if t.eng == "pe" and eng == "pe":
                continue
            assert t.val is not None, f"dependency on unsignalled op ({t.eng}->{eng})"
            if wd.get(t.sem, 0) < t.val:
                wd[t.sem] = t.val
                st.append(("wait", t.sem, t.val))

    def _mark(self, tok, reads, writes, key):
        for r in reads:
            r.r[key] = tok
        for w in writes:
            w.w = tok
            w.r = {}

    def op(self, eng, fn, reads=(), writes=(), signal=True):
        self.n_ops += 1
        self._emit_waits(eng, self._deps(reads, writes))
        tok = Tok(None, None, eng)
        if signal:
            c = self.cur[eng]
            if c[1] >= self.ROT:
                c[0], c[1] = self._new_sem(), 0
            c[1] += 1
            tok.sem, tok.val = c[0], c[1]
            for p in self.pending[eng]:
                p.sem, p.val = tok.sem, tok.val
            self.pending[eng] = []
            self.streams[eng].append(("op", fn, tok.sem, 1))
        else:
            self.pending[eng].append(tok)
            self.streams[eng].append(("op", fn, None, 0))
        self._mark(tok, reads, writes, ("e", eng))
        return tok

    def dma(self_, q, fn, reads=(), writes=()):
        self = self_
        self.n_ops += 1
        k = self.dma_rr[q]
        self.dma_rr[q] = (k + 1) % len(self.dsem[q])
        sem, cnt = self.dsem[q][k]
        deps = self._deps(reads, writes)
        if cnt > 0:
            deps.append(Tok(sem, cnt * 16, "dma"))
        self._emit_waits(q, deps)
        self.dsem[q][k][1] = cnt + 1
        tok = Tok(sem, (cnt + 1) * 16, "dma")
        self.streams[q].append(("op", fn, sem, 16))
        self._mark(tok, reads, writes, ("d", sem))
        return tok

    def wait_all(self, eng, resources):
        deps = []
        for r in resources:
            if r.w is not None:
                deps.append(r.w)
            deps.extend(r.r.values())
        self._emit_waits(eng, deps)

    def barrier(self):
        toks = []
        for e in self.COMPUTE:
            assert not self.pending[e], f"barrier with unsignalled ops on {e}"
            c = self.cur[e]
            if c[1] > 0:
                toks.append(Tok(c[0], c[1], e + "_b"))
        for q in self.dsem:
            for sem, cnt in self.dsem[q]:
                if cnt > 0:
                    toks.append(Tok(sem, cnt * 16, "dma"))
        for eng in self.streams:
            self._emit_waits(eng, toks)

    def emit(self, nc, stack):
        for e in self.COMPUTE:
            assert not self.pending[e], f"unsignalled trailing ops on {e}"
        sems = [stack.enter_context(nc.semaphore(f"s{i}")) for i in range(self.n_sems)]
        engs = {"pe": "tensor", "dve": "vector", "act": "scalar", "pool": "gpsimd", "sp": "sync"}
        block = stack.enter_context(nc.Block())
        for e, attr in engs.items():
            stream = self.streams[e]

            def body(engine, stream=stream):
                for item in stream:
                    if item[0] == "wait":
                        engine.wait_ge(sems[item[1]], item[2])
                    else:
                        ins = item[1](engine)
                        if item[2] is not None:
                            ins.then_inc(sems[item[2]], item[3])

            getattr(block, attr)(body)


class Builder:
    def __init__(self, nc, stack):
        self.nc = nc
        self.stack = stack
        self.s = Sched()
        self.uid = 0

    def sb(self, shape, dtype, name=None):
        self.uid += 1
        return self.stack.enter_context(self.nc.sbuf_tensor(name or f"sb{self.uid}", list(shape), dtype))

    def ps(self, shape, dtype, name=None):
        self.uid += 1
        return self.stack.enter_context(self.nc.psum_tensor(name or f"ps{self.uid}", list(shape), dtype))


    def dma(self, q, out, in_, reads=(), writes=(), **kw):
        return self.s.dma(q, lambda e: e.dma_start(out=out, in_=in_, **kw), reads, writes)

    def act(self, out, in_, func, reads=(), writes=(), **kw):
        return self.s.op("act", lambda e: e.activation(out=out, in_=in_, func=func, **kw), reads, writes)

    def mm(self, out, lhsT, rhs, start, stop, reads=(), writes=(), signal=None):
        return self.s.op("pe", lambda e: e.matmul(out, lhsT=lhsT, rhs=rhs, start=start, stop=stop), reads, writes,
                         signal=stop if signal is None else signal)

    def tr(self, out, in_, ident, reads=(), writes=(), signal=True):
        return self.s.op("pe", lambda e: e.transpose(out=out, in_=in_, identity=ident), reads, writes, signal=signal)

    def tt(self, out, in0, in1, op, reads=(), writes=(), eng="dve"):
        return self.s.op(eng, lambda e: e.tensor_tensor(out=out, in0=in0, in1=in1, op=op), reads, writes)

    def ts(self, out, in0, s1, s2, op0, op1=None, reads=(), writes=(), eng="dve", **kw):
        if op1 is None:
            return self.s.op(eng, lambda e: e.tensor_scalar(out=out, in0=in0, scalar1=s1, scalar2=None, op0=op0, **kw), reads, writes)
        return self.s.op(eng, lambda e: e.tensor_scalar(out=out, in0=in0, scalar1=s1, scalar2=s2, op0=op0, op1=op1, **kw), reads, writes)

    def stt(self, out, in0, scalar, in1, op0, op1, reads=(), writes=(), eng="dve"):
        return self.s.op(eng, lambda e: e.scalar_tensor_tensor(out=out, in0=in0, scalar=scalar, in1=in1, op0=op0, op1=op1),
                         reads, writes)

    def copy(self, out, in_, reads=(), writes=(), eng="dve"):
        if eng == "act":
            return self.s.op("act", lambda e: e.activation(out=out, in_=in_, func=AF.Copy), reads, writes)
        return self.s.op(eng, lambda e: e.tensor_copy(out=out, in_=in_), reads, writes)

    def recip(self, out, in_, reads=(), writes=()):
        return self.s.op("dve", lambda e: e.reciprocal(out=out, in_=in_), reads, writes)

    def memset(self, out, val, writes=(), eng="dve"):
        return self.s.op(eng, lambda e: e.memset(out, val), (), writes)


def ffn_stage(B, h_dram, h_res, norm_w, w1, w3, w2, T, st, n_tok=S):
    s = B.s
    TC = T // 128
    CB = st["CB"]
    NCB = DFF // CB
    FPB = CB // 128
    W2B = st["W2B"]
    NW2 = D // W2B
    ident = st["ident"]
    half = NFF // 2

    nw, nw_r = st["nw"], st["nw_r"]
    B.dma("sp", nw[:, :], norm_w.partition_broadcast(128), writes=[nw_r])

    w1v = w1.rearrange("(k p) f -> p k f", p=128)
    w3v = w3.rearrange("(k p) f -> p k f", p=128)
    w2v = w2.rearrange("(j p) d -> p j d", p=128)

    def load_w13(tile_idx, cb):
        slot = tile_idx % 2
        wt, wr = st["w13"][slot], st["w13_r"][slot]
        c0 = cb * CB
        B.dma("pool", wt[:, 0, :, :], w1v[:, :, c0:c0 + CB], writes=[wr[0]])
        B.dma("pool", wt[:, 1, :, :], w3v[:, :, c0:c0 + CB], writes=[wr[1]])

    def load_w2(tile_idx, nb):
        slot = tile_idx % 2
        wt, wr = st["w2"][slot], st["w2_r"][slot]
        c0 = nb * W2B
        B.dma("pool", wt[:, 0:half, :], w2v[:, 0:half, c0:c0 + W2B], writes=[wr[0]])
        B.dma("pool", wt[:, half:NFF, :], w2v[:, half:NFF, c0:c0 + W2B], writes=[wr[1]])

    n_tiles = n_tok // T
    w13_ctr = 0
    w2_ctr = 0
    ht, ht_r = st["ht"], st["ht_r"]
    xT, xT_r = st["xT"], st["xT_r"]
    gT, gT_r = st["gT"], st["gT_r"]
    hx, hx_r = st["hx"], st["hx_r"]

    def stats(src, src_r, c):
        sq, sq_r = st["sq"], st["sq_r"]
        ss, ss_r = st["ss"][c % 2], st["ss_r"][c % 2]
        xs, xs_r = st["xs"][c % 2], st["xs_r"][c % 2]
        B.act(sq[:, :], src, AF.Square, reads=[src_r], writes=[sq_r, ss_r], accum_out=ss[:, 0:1])
        B.act(ss[:, 1:2], ss[:, 0:1], AF.Sqrt, reads=[ss_r, st["eps_r"]], writes=[ss_r], scale=1.0 / D, bias=st["eps"][:, 0:1])
        B.recip(ss[:, 2:3], ss[:, 1:2], reads=[ss_r], writes=[ss_r])
        B.stt(xs[:, :], src, ss[:, 2:3], nw[:, :], ALU.mult, ALU.mult, reads=[src_r, ss_r, nw_r], writes=[xs_r])

    def transposes(c):
        xs, xs_r = st["xs"][c % 2], st["xs_r"][c % 2]
        for g in range(KC // 8):
            tp, tp_r = st["tp"][g % 2], st["tp_r"][g % 2]
            for k8 in range(8):
                k = g * 8 + k8
                B.tr(tp[:, k8, :], xs[:, k * 128:(k + 1) * 128], ident[:, :], reads=[xs_r], writes=[tp_r], signal=(k8 == 7))

    def evac(c):
        for g in range(KC // 8):
            tp, tp_r = st["tp"][g % 2], st["tp_r"][g % 2]
            B.copy(xT[:, g * 8:(g + 1) * 8, c * 128:(c + 1) * 128], tp[:, :, :], reads=[tp_r],
                   writes=xT_r[g * 8:(g + 1) * 8], eng="act" if g == 0 else "dve")

    for ti in range(n_tiles):
        t0 = ti * T
        for c in range(TC):
            B.dma("sp", ht[:, c, :], h_dram[t0 + c * 128:t0 + (c + 1) * 128, :], reads=[h_res[ti * TC + c]], writes=[ht_r[c]])
        if ti == 0:
            load_w13(w13_ctr, 0)
            stats(ht[:, 0, :], ht_r[0], 0)
            for c in range(TC):
                transposes(c)
                if c + 1 < TC:
                    stats(ht[:, c + 1, :], ht_r[c + 1], c + 1)
                evac(c)
        for cb in range(NCB):
            if cb + 1 < NCB:
                load_w13(w13_ctr + 1, cb + 1)
            else:
                load_w2(w2_ctr, 0)
            slot = w13_ctr % 2
            wt, wr = st["w13"][slot], st["w13_r"][slot]
            for f in range(FPB):
                j = cb * FPB + f
                pa, pa_r = st["pa"][j % 2], st["pa_r"][j % 2]
                pb, pb_r = st["pb"][j % 2], st["pb_r"][j % 2]
                sg, sg_r = st["sg"][j % 2], st["sg_r"][j % 2]
                for (which, pp, pp_r) in ((0, pa, pa_r), (1, pb, pb_r)):
                    for k in range(KC):
                        B.mm(pp[:, 0:T], wt[:, which, k, f * 128:(f + 1) * 128], xT[:, k, 0:T], k == 0, k == KC - 1,
                             reads=[wr[which], xT_r[k]], writes=[pp_r])
                B.act(sg[:, 0:T], pa[:, 0:T], AF.Silu, reads=[pa_r], writes=[sg_r])
                B.tt(gT[:, j, 0:T], sg[:, 0:T], pb[:, 0:T], ALU.mult, reads=[pb_r, sg_r], writes=[gT_r[j]])
            w13_ctr += 1
        pre = ti + 1 < n_tiles and NW2 >= 2 * TC
        for nb in range(NW2):
            if nb + 1 < NW2:
                load_w2(w2_ctr + 1, nb + 1)
            if pre:
                if nb == 0:
                    load_w13(w13_ctr, 0)
                c, odd = nb // 2, nb % 2
                if c < TC and not odd:
                    r0 = t0 + T + c * 128
                    B.dma("sp", hx[:, :], h_dram[r0:r0 + 128, :], reads=[h_res[(ti + 1) * TC + c]], writes=[hx_r])
                    stats(hx[:, :], hx_r, c)
                elif c < TC:
                    transposes(c)
                    evac(c)
            slot = w2_ctr % 2
            wt, wr = st["w2"][slot], st["w2_r"][slot]
            for c in range(TC):
                po, po_r = st["po"][(nb * TC + c) % 2], st["po_r"][(nb * TC + c) % 2]
                for j in range(NFF):
                    B.mm(po[:, 0:W2B], gT[:, j, c * 128:(c + 1) * 128], wt[:, j, :], j == 0, j == NFF - 1,
                         reads=[gT_r[j], wr[0 if j < half else 1]], writes=[po_r])
                hs = ht[:, c, nb * W2B:(nb + 1) * W2B]
                B.stt(hs, po[:, 0:W2B], 0.5, hs, ALU.mult, ALU.add, reads=[po_r, ht_r[c]], writes=[ht_r[c]])
            w2_ctr += 1
        for c in range(TC):
            B.dma("sp", h_dram[t0 + c * 128:t0 + (c + 1) * 128, :], ht[:, c, :], reads=[ht_r[c]], writes=[h_res[ti * TC + c]])


def alloc_ffn_state(B, T, ident):
    CB, W2B = 256, 256
    TC = T // 128
    st = {"CB": CB, "W2B": W2B, "ident": ident}

    def rl(n, name):
        return [Res(f"{name}{i}") for i in range(n)]

    st["nw"] = B.sb([128, D], F32); st["nw_r"] = Res("nw")
    st["eps"] = B.sb([128, 1], F32)
    st["eps_r"] = Res("eps")
    B.s.op("dve", lambda e: e.memset(st["eps"][:, :], EPS), writes=[st["eps_r"]])
    st["ht"] = B.sb([128, TC, D], F32); st["ht_r"] = rl(TC, "ht")
    st["hx"] = B.sb([128, D], F32); st["hx_r"] = Res("hx")
    st["sq"] = B.sb([128, D], BF16); st["sq_r"] = Res("sq")
    st["ss"] = [B.sb([128, 4], F32) for _ in range(2)]; st["ss_r"] = rl(2, "ss")
    st["xs"] = [B.sb([128, D], BF16) for _ in range(2)]; st["xs_r"] = rl(2, "xs")
    st["xT"] = B.sb([128, KC, T], BF16); st["xT_r"] = rl(KC, "xT")
    st["gT"] = B.sb([128, NFF, T], BF16); st["gT_r"] = rl(NFF, "gT")
    st["w13"] = [B.sb([128, 2, KC, CB], BF16) for _ in range(2)]; st["w13_r"] = [rl(2, f"w13_{i}") for i in range(2)]
    st["w2"] = [B.sb([128, NFF, W2B], BF16) for _ in range(2)]; st["w2_r"] = [rl(2, f"w2_{i}") for i in range(2)]
    st["sg"] = [B.sb([128, T], F32) for _ in range(2)]; st["sg_r"] = rl(2, "sg")
    st["tp"] = [B.ps([128, 8, 128], BF16) for _ in range(2)]; st["tp_r"] = rl(2, "tp")
    st["pa"] = [B.ps([128, 512], F32) for _ in range(2)]; st["pa_r"] = rl(2, "pa")
    st["pb"] = [B.ps([128, 512], F32) for _ in range(2)]; st["pb_r"] = rl(2, "pb")
    st["po"] = [B.ps([128, 512], F32) for _ in range(2)]; st["po_r"] = rl(2, "po")
    return st


HM, HR, HH = 8, 4, 4
C_MQ, C_CKV, C_KR, C_RQ, C_RK, C_RV, C_RG, C_HQ, C_HF, C_HI, C_HG = (
    0, 1536, 2048, 2112, 2624, 3136, 3648, 4160, 4672, 5184, 5696)
MLA_SCALE = 192.0 ** -0.5


def rl(n, name):
    return [Res(f"{name}{i}") for i in range(n)]


def round_robin(gens):
    gens = list(gens)
    while gens:
        for g in list(gens):
            try:
                next(g)
            except StopIteration:
                gens.remove(g)


def proj_stage(B, h_dram, h_res, norm_w, w_in, kvn_w, cst, scr, n_tok=S):
    s = B.s
    NT = n_tok // 128
    NB = max(1, n_tok // 512)
    TB = min(512, n_tok)
    with ExitStack() as sub:
        B.stack, old = sub, B.stack
        ident = cst["ident"]
        nw = B.sb([128, D], F32); nw_r = Res()
        kw = B.sb([128, 4], F32); kw_r = Res()
        B.dma("sp", nw[:, :], norm_w.partition_broadcast(128), writes=[nw_r])
        B.dma("sp", kw[:, :], kvn_w.rearrange("(k p) -> p k", p=128), writes=[kw_r], allow_slow_non_contiguous=True)
        xT = B.sb([128, KC, n_tok], BF16); xT_r = rl(KC, "xT")
        ht = [B.sb([128, D], F32) for _ in range(3)]; ht_r = rl(3, "ht")
        sq = B.sb([128, D], BF16); sq_r = Res()
        ss = [B.sb([128, 4], F32) for _ in range(2)]; ss_r = rl(2, "ss")
        xs = [B.sb([128, D], BF16) for _ in range(2)]; xs_r = rl(2, "xs")
        tp = [B.ps([128, 8, 128], BF16) for _ in range(2)]; tp_r = rl(2, "tp")
        pp = [B.ps([128, 512], F32) for _ in range(4)]; pp_r = rl(4, "pp")
        wb = [B.sb([128, KC, 512], BF16) for _ in range(2)]; wb_r = [rl(4, f"wb{i}_") for i in range(2)]
        ws = [B.sb([128, KC, 128], BF16) for _ in range(2)]; ws_r = [rl(2, f"ws{i}_") for i in range(2)]
        tabs = {"m": (cst["cosm"], cst["sinm"]), "r": (cst["cosr"], cst["sinr"])}
        stg = [B.sb([128, 512], BF16) for _ in range(3)]; stg_r = rl(3, "stg")
        stf = [B.sb([128, 512], F32) for _ in range(2)]; stf_r = rl(2, "stf")
        t1 = [B.sb([128, 512], F32) for _ in range(2)]; t1_r = rl(2, "t1")
        t2 = [B.sb([128, 512], F32) for _ in range(2)]; t2_r = rl(2, "t2")
        eps, eps_r = cst["eps"], cst["eps_r"]

        for c in range(min(2, NT)):
            B.dma("sp", ht[c % 3][:, :], h_dram[c * 128:(c + 1) * 128, :], reads=[h_res[c]], writes=[ht_r[c % 3]])

        def stats(c):
            b, h4 = c % 2, c % 3
            if c + 2 < NT:
                B.dma("sp", ht[(c + 2) % 3][:, :], h_dram[(c + 2) * 128:(c + 3) * 128, :], reads=[h_res[c + 2]], writes=[ht_r[(c + 2) % 3]])
            B.act(sq[:, :], ht[h4][:, :], AF.Square, reads=[ht_r[h4]], writes=[sq_r, ss_r[b]], accum_out=ss[b][:, 0:1])
            B.act(ss[b][:, 1:2], ss[b][:, 0:1], AF.Sqrt, reads=[ss_r[b], eps_r], writes=[ss_r[b]], scale=1.0 / D, bias=eps[:, 0:1])
            B.recip(ss[b][:, 2:3], ss[b][:, 1:2], reads=[ss_r[b]], writes=[ss_r[b]])
            B.stt(xs[b][:, :], ht[h4][:, :], ss[b][:, 2:3], nw[:, :], ALU.mult, ALU.mult, reads=[ht_r[h4], ss_r[b], nw_r], writes=[xs_r[b]])

        stats(0)
        for c in range(NT):
            b = c % 2
            for g in range(KC // 8):
                for k8 in range(8):
                    k = g * 8 + k8
                    B.tr(tp[g][:, k8, :], xs[b][:, k * 128:(k + 1) * 128], ident[:, :], reads=[xs_r[b]], writes=[tp_r[g]], signal=(k8 == 7))
            if c + 1 < NT:
                stats(c + 1)
            for g in range(KC // 8):
                B.copy(xT[:, g * 8:(g + 1) * 8, c * 128:(c + 1) * 128], tp[g][:, :, :], reads=[tp_r[g]],
                       writes=xT_r[g * 8:(g + 1) * 8], eng="act" if g == 0 else "dve")

        wv = w_in.rearrange("(k p) f -> p k f", p=128)
        ctr = {"wb": 0, "ws": 0, "pp": 0, "stg": 0, "stf": 0, "t": 0}

        def load_block(c0, ncols):
            i = ctr["wb"] % 2; ctr["wb"] += 1
            nq = (ncols + 127) // 128
            for q in range(nq):
                w_ = min(128, ncols - q * 128)
                B.dma("pool", wb[i][:, :, q * 128:q * 128 + w_], wv[:, :, c0 + q * 128:c0 + q * 128 + w_], writes=[wb_r[i][q]])
            return i

        def load_swapped(c0, n):
            i = ctr["ws"] % 2; ctr["ws"] += 1
            hf = n // 2
            B.dma("pool", ws[i][:, :, 0:hf], wv[:, :, c0 + hf:c0 + n], writes=[ws_r[i][0]])
            B.dma("pool", ws[i][:, :, hf:n], wv[:, :, c0:c0 + hf], writes=[ws_r[i][1]])
            return i

        def mm_fm(psum, psum_r, wt, wt_rs, M, tb):
            for k in range(KC):
                B.mm(psum[0:M, 0:TB], wt[:, k, :], xT[:, k, tb * TB:(tb + 1) * TB], k == 0, k == KC - 1,
                     reads=list(wt_rs) + [xT_r[k]], writes=[psum_r])

        def next_pp():
            i = ctr["pp"] % 4; ctr["pp"] += 1
            return pp[i], pp_r[i]

        def fm_tile(bi, q, M, kind, dst, tab=None, swi=None):
            wt = wb[bi][:, :, q * 128:q * 128 + M]
            for tb in range(NB):
                pa, pa_r = next_pp()
                mm_fm(pa, pa_r, wt, [wb_r[bi][q]], M, tb)
                dsl = dst[0:M, tb * TB:(tb + 1) * TB]
                if kind == "rope":
                    pb, pb_r = next_pp()
                    mm_fm(pb, pb_r, ws[swi][:, :, 0:M], ws_r[swi], M, tb)
                    ct, sn = tabs[tab]
                    j = ctr["t"] % 2; ctr["t"] += 1
                    B.tt(t1[j][0:M, 0:TB], pa[0:M, 0:TB], ct[0:M, tb * TB:(tb + 1) * TB], ALU.mult, reads=[pa_r], writes=[t1_r[j]])
                    B.tt(t2[j][0:M, 0:TB], pb[0:M, 0:TB], sn[0:M, tb * TB:(tb + 1) * TB], ALU.mult, reads=[pb_r], writes=[t2_r[j]])
                    g = ctr["stg"] % 3; ctr["stg"] += 1
                    B.tt(stg[g][0:M, 0:TB], t1[j][0:M, 0:TB], t2[j][0:M, 0:TB], ALU.add, reads=[t1_r[j], t2_r[j]], writes=[stg_r[g]])
                    B.dma("sp", dsl, stg[g][0:M, 0:TB], reads=[stg_r[g]])
                elif kind == "f32":
                    g = ctr["stf"] % 2; ctr["stf"] += 1
                    B.copy(stf[g][0:M, 0:TB], pa[0:M, 0:TB], reads=[pa_r], writes=[stf_r[g]], eng="dve")
                    B.dma("sp", dsl, stf[g][0:M, 0:TB], reads=[stf_r[g]])
                else:
                    g = ctr["stg"] % 3; ctr["stg"] += 1
                    B.act(stg[g][0:M, 0:TB], pa[0:M, 0:TB], AF.Silu if kind == "silu" else AF.Copy, reads=[pa_r], writes=[stg_r[g]])
                    B.dma("sp", dsl, stg[g][0:M, 0:TB], reads=[stg_r[g]])

        for h in range(HM):
            c0 = C_MQ + h * 192
            bi = load_block(c0, 192)
            swi = load_swapped(c0 + 128, 64)
            fm_tile(bi, 0, 128, "copy", scr["QN"][h])
            fm_tile(bi, 1, 64, "rope", scr["QR"][h], tab="m", swi=swi)
        bi = load_block(C_KR, 64)
        swi = load_swapped(C_KR, 64)
        fm_tile(bi, 0, 64, "rope", scr["KR"], tab="m", swi=swi)
        for (c0, dst) in ((C_RQ, scr["RQ"]), (C_RK, scr["RK"])):
            bi = load_block(c0, 512)
            for h in range(HR):
                swi = load_swapped(c0 + h * 128, 128)
                fm_tile(bi, h, 128, "rope", dst[h], tab="r", swi=swi)
        for (c0, dst, kind) in ((C_RG, scr["RG"], "silu"), (C_HQ, scr["HQ"], "f32"), (C_HF, scr["HF"], "f32"),
                                (C_HG, scr["HG"], "silu")):
            bi = load_block(c0, 512)
            for h in range(4):
                fm_tile(bi, h, 128, kind, dst[h])
        for (c0, dst) in ((C_RV, scr["RV"]), (C_HI, scr["HI"])):
            bi = load_block(c0, 512)
            for c in range(NT):
                pa, pa_r = next_pp()
                for k in range(KC):
                    B.mm(pa[:, 0:512], xT[:, k, c * 128:(c + 1) * 128], wb[bi][:, k, :], k == 0, k == KC - 1,
                         reads=wb_r[bi] + [xT_r[k]], writes=[pa_r])
                g = ctr["stg"] % 3; ctr["stg"] += 1
                B.copy(stg[g][:, :], pa[:, 0:512], reads=[pa_r], writes=[stg_r[g]], eng="act" if c % 2 else "dve")
                B.dma("sp", dst[c * 128:(c + 1) * 128, :], stg[g][:, :], reads=[stg_r[g]])
        bi = load_block(C_CKV, 512)
        for c in range(NT):
            b = c % 2
            pa, pa_r = next_pp()
            for k in range(KC):
                B.mm(pa[:, 0:512], xT[:, k, c * 128:(c + 1) * 128], wb[bi][:, k, :], k == 0, k == KC - 1,
                     reads=wb_r[bi] + [xT_r[k]], writes=[pa_r])
            B.act(sq[:, 0:512], pa[:, 0:512], AF.Square, reads=[pa_r], writes=[sq_r, ss_r[b]], accum_out=ss[b][:, 0:1])
            B.act(ss[b][:, 1:2], ss[b][:, 0:1], AF.Sqrt, reads=[ss_r[b], eps_r], writes=[ss_r[b]], scale=1.0 / 512, bias=eps[:, 0:1])
            B.recip(ss[b][:, 2:3], ss[b][:, 1:2], reads=[ss_r[b]], writes=[ss_r[b]])
            B.ts(xs[b][:, 0:512], pa[:, 0:512], ss[b][:, 2:3], None, ALU.mult, reads=[pa_r, ss_r[b]], writes=[xs_r[b]])
            g2 = c % 2
            for k4 in range(4):
                B.tr(tp[g2][:, k4, :], xs[b][:, k4 * 128:(k4 + 1) * 128], ident[:, :], reads=[xs_r[b]], writes=[tp_r[g2]], signal=(k4 == 3))
            g = ctr["stg"] % 3; ctr["stg"] += 1
            for k4 in range(4):
                B.act(stg[g][:, k4 * 128:(k4 + 1) * 128], tp[g2][:, k4, :], AF.Copy, reads=[tp_r[g2], kw_r], writes=[stg_r[g]], scale=kw[:, k4:k4 + 1])
            B.dma("sp", scr["CKVN"][:, :, c * 128:(c + 1) * 128].rearrange("k p t -> p k t"),
                  stg[g][:, :].rearrange("p (k t) -> p k t", k=4), reads=[stg_r[g]])
        s.barrier()
        B.stack = old


def mla_stage(B, wkvb, onw, cst, scr, n_tok=S):
    s = B.s
    NT = n_tok // 128
    with ExitStack() as sub:
        B.stack, old = sub, B.stack
        ident = cst["ident"]
        eps, eps_r = cst["eps"], cst["eps_r"]
        maskneg = cst["maskneg"]
        c_r = cst["c_r"]
        ckv = B.sb([128, 4, n_tok], BF16); ckv_r = Res()
        krT = B.sb([64, n_tok], BF16); kr_r = Res()
        B.dma("sp", ckv[:, :, :], scr["CKVN"][:, :, 0:n_tok].rearrange("k p t -> p k t"), writes=[ckv_r])
        B.dma("sp", krT[:, :], scr["KR"][:, 0:n_tok], writes=[kr_r])
        ow = B.sb([128, HM], F32); ow_r = Res()
        B.dma("sp", ow[:, :], onw.rearrange("(h p) -> p h", p=128), writes=[ow_r], allow_slow_non_contiguous=True)
        wk = [B.sb([128, 4, 256], BF16) for _ in range(2)]; wk_r = rl(2, "wk")
        qn = [B.sb([128, n_tok], BF16) for _ in range(2)]; qn_r = rl(2, "qn")
        qr = [B.sb([64, n_tok], BF16) for _ in range(2)]; qr_r = rl(2, "qr")
        kn = [B.sb([128, n_tok], BF16) for _ in range(2)]; kn_r = rl(2, "kn")
        vv = [B.sb([128, NT, 128], BF16) for _ in range(2)]; vv_r = rl(2, "vv")
        oT = [B.sb([128, n_tok], BF16) for _ in range(2)]; oT_r = rl(2, "oT")
        pr = [B.sb([128, n_tok], BF16) for _ in range(2)]; pr_r = rl(2, "pr")
        pT = [B.sb([128, NT, 128], BF16) for _ in range(2)]; pT_r = rl(2, "pT")
        mx = [B.sb([128, 2], F32) for _ in range(2)]; mx_r = rl(2, "mx")
        oacc = [B.sb([128, NT, 128], F32) for _ in range(2)]; oacc_r = rl(2, "oacc")
        ssum = [B.sb([128, NT, 1], F32) for _ in range(2)]; ssum_r = rl(2, "ssum")
        nst = B.sb([128, 3, NT], F32); nst_r = Res()
        sqb = B.sb([128, NT, 128], F32); sqb_r = Res()
        o2 = B.sb([128, NT, 128], BF16); o2_r = Res()
        scA = B.ps([128, 2048], F32); scB = B.ps([128, 1024], F32)
        sc = [scA, scB]; sc_rb = [rl(4, "scA"), rl(2, "scB")]
        scb16 = [scA[:, :].bitcast(BF16), scB[:, :].bitcast(BF16)]
        pX = B.ps([128, 512], F32); pY = B.ps([128, 512], F32)
        pxy = [pX, pY]; pxy_r = rl(2, "pxy")
        pYb = pY[:, :].bitcast(BF16)
        wv = wkvb.rearrange("(k p) f -> p k f", p=128)

        def head_prep(h):
            b = h % 2
            B.dma("pool", wk[b][:, :, :], wv[:, :, h * 256:(h + 1) * 256], writes=[wk_r[b]])
            B.dma("sp", qn[b][:, :], scr["QN"][h][:, 0:n_tok], writes=[qn_r[b]])
            B.dma("sp", qr[b][:, :], scr["QR"][h][:, 0:n_tok], writes=[qr_r[b]])
            for t2 in range(n_tok // 256):
                ts_ = slice(t2 * 256, (t2 + 1) * 256)
                for k in range(4):
                    B.mm(pX[:, 0:256], wk[b][:, k, 0:128], ckv[:, k, ts_], k == 0, k == 3, reads=[wk_r[b], ckv_r], writes=[pxy_r[0]])
                for cc in range(2):
                    c = t2 * 2 + cc
                    for k in range(4):
                        B.mm(pX[:, 256 + cc * 128:256 + (cc + 1) * 128], ckv[:, k, c * 128:(c + 1) * 128], wk[b][:, k, 128:256],
                             k == 0, k == 3, reads=[wk_r[b], ckv_r], writes=[pxy_r[0]], signal=(k == 3 and cc == 1))
                B.copy(kn[b][:, ts_], pX[:, 0:256], reads=[pxy_r[0]], writes=[kn_r[b]], eng="act")
                B.copy(vv[b][:, t2 * 2:t2 * 2 + 2, :], pX[:, 256:512].rearrange("p (c d) -> p c d", d=128),
                       reads=[pxy_r[0]], writes=[vv_r[b]], eng="act")

        order = []
        for a in range(NT // 2):
            order += [NT - 1 - a, a]
        steps = [(h, i) for h in range(HM) for i in order]

        def scores(t):
            h, i = steps[t]
            b, u = h % 2, t % 2
            q0 = i * 128
            for g0 in range(0, q0, 512):
                g1 = min(g0 + 512, q0)
                wr = [sc_rb[u][g0 // 512]]
                B.mm(sc[u][:, g0:g1], qn[b][:, q0:q0 + 128], kn[b][:, g0:g1], True, False, reads=[qn_r[b], kn_r[b]], writes=wr, signal=False)
                B.mm(sc[u][:, g0:g1], qr[b][:, q0:q0 + 128], krT[:, g0:g1], False, True, reads=[qr_r[b], kr_r], writes=wr, signal=False)
            wr = [sc_rb[u][q0 // 512]]
            B.mm(sc[u][:, q0:q0 + 128], qn[b][:, q0:q0 + 128], kn[b][:, q0:q0 + 128], True, False, reads=[qn_r[b], kn_r[b]], writes=wr, signal=False)
            B.mm(sc[u][:, q0:q0 + 128], qr[b][:, q0:q0 + 128], krT[:, q0:q0 + 128], False, False, reads=[qr_r[b], kr_r], writes=wr, signal=False)
            B.mm(sc[u][:, q0:q0 + 128], ident[:, :], maskneg[:, :], False, True, reads=[c_r], writes=wr, signal=True)

        def softmax(t):
            h, i = steps[t]
            b, u = h % 2, t % 2
            nk = (i + 1) * 128
            banks = sc_rb[u][0:(nk + 511) // 512]
            B.s.op("dve", lambda e, o=mx[u][:, 0:1], a=sc[u][:, 0:nk]: e.reduce_max(out=o, in_=a, axis=AX.X), reads=banks, writes=[mx_r[u]])
            B.ts(mx[u][:, 1:2], mx[u][:, 0:1], -MLA_SCALE, None, ALU.mult, reads=[mx_r[u]], writes=[mx_r[u]])
            B.act(pr[u][:, 0:nk], sc[u][:, 0:nk], AF.Exp, reads=banks + [mx_r[u]], writes=[pr_r[u], ssum_r[b]],
                  scale=MLA_SCALE, bias=mx[u][:, 1:2], accum_out=ssum[b][:, i, :])

        def pv(t):
            h, i = steps[t]
            b, u = h % 2, t % 2
            for c8 in range((i + 8) // 8):
                ncs = min(8, i + 1 - c8 * 8)
                for cc in range(ncs):
                    c = c8 * 8 + cc
                    B.tr(scb16[u][:, c * 128:(c + 1) * 128], pr[u][:, c * 128:(c + 1) * 128], ident[:, :], reads=[pr_r[u]],
                         writes=[sc_rb[u][c8]], signal=(cc == ncs - 1))
                B.copy(pT[u][:, c8 * 8:c8 * 8 + ncs, :], scb16[u][:, c8 * 1024:c8 * 1024 + ncs * 128].rearrange("p (c q) -> p c q", q=128),
                       reads=[sc_rb[u][c8]], writes=[pT_r[u]], eng="dve")
            for c in range(i + 1):
                B.mm(pxy[u][:, 0:128], pT[u][:, c, :], vv[b][:, c, :], c == 0, c == i, reads=[pT_r[u], vv_r[b]], writes=[pxy_r[u]])
            B.copy(oacc[b][:, i, :], pxy[u][:, 0:128], reads=[pxy_r[u]], writes=[oacc_r[b]], eng="dve")

        def head_norm(h):
            b = h % 2
            o3 = oacc[b][:, :, :]
            B.recip(nst[:, 0, :], ssum[b][:, :, :].rearrange("p n o -> p (n o)"), reads=[ssum_r[b]], writes=[nst_r])
            B.tt(o3, o3, nst[:, 0, :].rearrange("p (n o) -> p n o", o=1).broadcast_to([128, NT, 128]), ALU.mult, reads=[nst_r, oacc_r[b]], writes=[oacc_r[b]])
            B.tt(sqb[:, :, :], o3, o3, ALU.mult, reads=[oacc_r[b]], writes=[sqb_r])
            B.s.op("dve", lambda e, o=nst[:, 1, :], a=sqb[:, :, :]: e.tensor_reduce(out=o, in_=a, axis=AX.X, op=ALU.add), reads=[sqb_r], writes=[nst_r])
            B.act(nst[:, 2, :], nst[:, 1, :], AF.Sqrt, reads=[nst_r, eps_r], writes=[nst_r], scale=1.0 / 128, bias=eps[:, 0:1])
            B.recip(nst[:, 2, :], nst[:, 2, :], reads=[nst_r], writes=[nst_r])
            B.tt(o2[:, :, :], o3, nst[:, 2, :].rearrange("p (n o) -> p n o", o=1).broadcast_to([128, NT, 128]), ALU.mult, reads=[nst_r, oacc_r[b]], writes=[o2_r])
            for c8 in range((NT + 7) // 8):
                ncs = min(8, NT - c8 * 8)
                for cc in range(ncs):
                    i = c8 * 8 + cc
                    B.tr(pYb[:, cc * 128:(cc + 1) * 128], o2[:, i, :], ident[:, :], reads=[o2_r], writes=[pxy_r[1]], signal=(cc == ncs - 1))
                B.act(oT[b][:, c8 * 1024:c8 * 1024 + ncs * 128].rearrange("p (c q) -> p c q", q=128),
                      pYb[:, 0:ncs * 128].rearrange("p (c q) -> p c q", q=128), AF.Copy,
                      reads=[pxy_r[1], ow_r], writes=[oT_r[b]], scale=ow[:, h:h + 1])
            B.dma("sp", scr["CAT"][h][:, 0:n_tok], oT[b][:, :], reads=[oT_r[b]])

        head_prep(0)
        scores(0)
        nsteps = len(steps)
        for t in range(nsteps):
            h, i = steps[t]
            pos = t % NT
            softmax(t)
            if pos == min(2, NT - 1) and h + 1 < HM:
                head_prep(h + 1)
            if t + 1 < nsteps:
                scores(t + 1)
            pv(t)
            if pos == NT - 1:
                head_norm(h)
        s.barrier()
        B.stack = old


def fm_norm(B, oall, oall_r, n_tok, mode, w_ap, w_r, gate, gate_r, dst, cst, bufs):
    TB = min(512, n_tok)
    ones = cst["ones"]
    eps, eps_r = cst["eps"], cst["eps_r"]
    ps1, ps1_r, ps2, ps2_r, dd, dd_r, sqb, sqb_r, rs, rs_r, og, og_r = bufs
    for tb in range(max(1, n_tok // 512)):
        sl = slice(tb * TB, (tb + 1) * TB)
        if mode == "gn":
            B.mm(ps1[:, 0:TB], ones[:, :], oall[:, sl], True, True, reads=[oall_r, cst["c_r"]], writes=[ps1_r])
            B.stt(dd[:, 0:TB], ps1[:, 0:TB], -1.0 / 128, oall[:, sl], ALU.mult, ALU.add, reads=[ps1_r, oall_r], writes=[dd_r])
            src = dd[:, 0:TB]
        else:
            src = oall[:, sl]
        B.tt(sqb[:, 0:TB], src, src, ALU.mult, reads=[dd_r, oall_r], writes=[sqb_r])
        B.mm(ps2[:, 0:TB], ones[:, :], sqb[:, 0:TB], True, True, reads=[sqb_r, cst["c_r"]], writes=[ps2_r])
        B.act(rs[:, 0:TB], ps2[:, 0:TB], AF.Sqrt, reads=[ps2_r, eps_r], writes=[rs_r], scale=1.0 / 128, bias=eps[:, 0:1])
        B.recip(rs[:, 0:TB], rs[:, 0:TB], reads=[rs_r], writes=[rs_r])
        B.tt(sqb[:, 0:TB], src, rs[:, 0:TB], ALU.mult, reads=[dd_r, oall_r, rs_r], writes=[sqb_r])
        B.stt(og[:, 0:TB], sqb[:, 0:TB], w_ap, gate[:, sl], ALU.mult, ALU.mult, reads=[sqb_r, w_r, gate_r], writes=[og_r])
        B.dma("sp", dst[:, sl], og[:, 0:TB], reads=[og_r])


def ret_stage(B, gnw, cst, scr, n_tok=S):
    s = B.s
    NT = n_tok // 128
    with ExitStack() as sub:
        B.stack, old = sub, B.stack
        ident = cst["ident"]
        c_r = cst["c_r"]
        gw = B.sb([128, HR], F32); gw_r = Res()
        B.dma("sp", gw[:, :], gnw.rearrange("(h p) -> p h", p=128), writes=[gw_r], allow_slow_non_contiguous=True)
        hb = []
        for j in range(2):
            d = {}
            d["qT"] = B.sb([128, n_tok], BF16); d["qT_r"] = Res()
            d["kT"] = B.sb([128, n_tok], BF16); d["kT_r"] = Res()
            d["gT"] = B.sb([128, n_tok], BF16); d["gT_r"] = Res()
            d["vv"] = B.sb([128, NT, 128], BF16); d["vv_r"] = Res()
            d["oall"] = B.sb([128, n_tok], F32); d["oall_r"] = Res()
            d["R"] = B.sb([128, 128], F32); d["R_r"] = Res()
            d["Rb"] = B.sb([128, 128], BF16); d["Rb_r"] = Res()
            d["AT"] = B.sb([128, 128], BF16); d["AT_r"] = Res()
            d["qx"] = B.sb([128, 128], BF16); d["qx_r"] = Res()
            d["kz"] = B.sb([128, 128], BF16); d["kz_r"] = Res()
            d["psS"] = B.ps([128, 512], F32); d["psS_r"] = Res()
            d["psO"] = B.ps([128, 512], F32); d["psO_r"] = Res()
            d["psU"] = B.ps([128, 512], F32); d["psU_r"] = Res()
            d["tp"] = B.ps([128, 8, 128], BF16); d["tp_r"] = Res()
            hb.append(d)
        nbs = B.sb([128, 512], F32), B.sb([128, 512], F32), B.sb([128, 512], F32), B.sb([128, 512], BF16)
        nbr = Res(), Res(), Res(), Res()

        def load(h, d):
            B.dma("sp", d["qT"][:, :], scr["RQ"][h][:, 0:n_tok], writes=[d["qT_r"]])
            B.dma("sp", d["kT"][:, :], scr["RK"][h][:, 0:n_tok], writes=[d["kT_r"]])
            B.dma("sp", d["gT"][:, :], scr["RG"][h][:, 0:n_tok], writes=[d["gT_r"]])
            B.dma("sp", d["vv"][:, :, :], scr["RV"][0:n_tok, h * 128:(h + 1) * 128].rearrange("(c p) e -> p c e", p=128), writes=[d["vv_r"]])

        def chain(h, d):
            gch = float(np.float32(np.exp(np.float32(np.log1p(-np.float32(2.0 ** (-5.0 - h)))) * np.float32(128.0))))
            qT, kT, vv = d["qT"], d["kT"], d["vv"]
            for n in range(NT):
                cs = slice(n * 128, (n + 1) * 128)
                B.mm(d["psS"][:, 0:128], kT[:, cs], qT[:, cs], True, True, reads=[d["kT_r"], d["qT_r"]], writes=[d["psS_r"]])
                if n < NT - 1:
                    B.tr(d["tp"][:, 0, :], kT[:, cs], ident[:, :], reads=[d["kT_r"]], writes=[d["tp_r"]])
                yield
                B.tt(d["AT"][:, :], d["psS"][:, 0:128], cst["dmT"][:, h, :], ALU.mult, reads=[d["psS_r"], c_r], writes=[d["AT_r"]])
                if n > 0:
                    B.tt(d["qx"][:, :], qT[:, cs], cst["xi"][:, h, :], ALU.mult, reads=[d["qT_r"], c_r], writes=[d["qx_r"]], eng="pool")
                if n < NT - 1:
                    B.act(d["kz"][:, :], d["tp"][:, 0, :], AF.Copy, reads=[d["tp_r"], c_r], writes=[d["kz_r"]], scale=cst["zeta"][:, h:h + 1])
                yield
                B.mm(d["psO"][:, 0:128], vv[:, n, :], d["AT"][:, :], True, n == 0, reads=[d["vv_r"], d["AT_r"]], writes=[d["psO_r"]])
                if n > 0:
                    B.mm(d["psO"][:, 0:128], d["Rb"][:, :], d["qx"][:, :], False, True, reads=[d["Rb_r"], d["qx_r"]], writes=[d["psO_r"]])
                if n < NT - 1:
                    B.mm(d["psU"][:, 0:128], d["kz"][:, :], vv[:, n, :], True, True, reads=[d["kz_r"], d["vv_r"]], writes=[d["psU_r"]])
                yield
                B.copy(d["oall"][:, cs], d["psO"][:, 0:128], reads=[d["psO_r"]], writes=[d["oall_r"]], eng="act")
                if n < NT - 1:
                    if n == 0:
                        B.copy(d["R"][:, :], d["psU"][:, 0:128], reads=[d["psU_r"]], writes=[d["R_r"]], eng="dve")
                    else:
                        B.stt(d["R"][:, :], d["R"][:, :], gch, d["psU"][:, 0:128], ALU.mult, ALU.add, reads=[d["psU_r"], d["R_r"]], writes=[d["R_r"]])
                    B.copy(d["Rb"][:, :], d["R"][:, :], reads=[d["R_r"]], writes=[d["Rb_r"]], eng="dve")
                yield

        for pair in range(HR // 2):
            hs = [2 * pair, 2 * pair + 1]
            for j, h in enumerate(hs):
                load(h, hb[j])
            round_robin([chain(h, hb[j]) for j, h in enumerate(hs)])
            for j, h in enumerate(hs):
                d = hb[j]
                nb = (d["psS"], d["psS_r"], d["psO"], d["psO_r"], nbs[0], nbr[0], nbs[1], nbr[1], nbs[2], nbr[2], nbs[3], nbr[3])
                fm_norm(B, d["oall"], d["oall_r"], n_tok, "gn", gw[:, h:h + 1], gw_r, d["gT"], d["gT_r"], scr["CAT"][8 + h], cst, nb)
        s.barrier()
        B.stack = old


def hgrn_stage(B, lb, lb_r, layer, hnw, cst, scr, n_tok=S):
    s = B.s
    NC = n_tok // 64
    with ExitStack() as sub:
        B.stack, old = sub, B.stack
        ident = cst["ident"]
        c_r = cst["c_r"]
        hw = B.sb([128, HH], F32); hw_r = Res()
        B.dma("sp", hw[:, :], hnw.rearrange("(h p) -> p h", p=128), writes=[hw_r], allow_slow_non_contiguous=True)
        q = B.sb([128, n_tok], F32); q_r = Res()
        f = B.sb([128, n_tok], F32); f_r = Res()
        kk = B.sb([128, n_tok], F32); kk_r = Res()
        bb = B.sb([128, n_tok], F32); bb_r = Res()
        e1 = B.sb([128, n_tok], F32); e1_r = Res()
        e2 = B.sb([128, n_tok], F32); e2_r = Res()
        hb = []
        for j in range(2):
            d = {}
            for nm in ("qt", "kt", "qh", "kh", "gT"):
                d[nm] = B.sb([128, n_tok], BF16); d[nm + "_r"] = Res()
            d["ebl"] = B.sb([128, NC], F32); d["ebl_r"] = Res()
            d["vv"] = B.sb([64, NC, 128], BF16); d["vv_r"] = Res()
            d["oall"] = B.sb([128, n_tok], F32); d["oall_r"] = Res()
            d["St"] = B.sb([128, 128], F32); d["St_r"] = Res()
            d["Sb"] = B.sb([128, 128], BF16); d["Sb_r"] = Res()
            d["ATm"] = B.sb([64, 64], BF16); d["ATm_r"] = Res()
            d["khT"] = B.sb([64, 128], BF16); d["khT_r"] = Res()
            d["psS"] = B.ps([128, 512], F32); d["psS_r"] = Res()
            d["psO"] = B.ps([128, 512], F32); d["psO_r"] = Res()
            d["psU"] = B.ps([128, 512], F32); d["psU_r"] = Res()
            d["tp"] = B.ps([128, 8, 128], BF16); d["tp_r"] = Res()
            hb.append(d)
        nbs = e1[:, 0:min(512, n_tok)], e2[:, 0:min(512, n_tok)], bb[:, 0:min(256, n_tok // 2)].bitcast(BF16)
        nbr = e1_r, e2_r, bb_r

        def prep(h, d):
            lbh = lb[:, layer, h:h + 1]
            omh = lb[:, 4 + layer, h:h + 1]
            B.dma("sp", q[:, :], scr["HQ"][h][:, 0:n_tok], writes=[q_r])
            B.dma("sp", f[:, :], scr["HF"][h][:, 0:n_tok], writes=[f_r])
            B.dma("sp", d["gT"][:, :], scr["HG"][h][:, 0:n_tok], writes=[d["gT_r"]])
            B.dma("sp", d["vv"][:, :, :], scr["HI"][0:n_tok, h * 128:(h + 1) * 128].rearrange("(c p) e -> p c e", p=64), writes=[d["vv_r"]])
            B.act(f[:, :], f[:, :], AF.Sigmoid, reads=[f_r], writes=[f_r])
            B.ts(f[:, :], f[:, :], omh, lbh, ALU.mult, ALU.add, reads=[f_r, lb_r], writes=[f_r])
            B.ts(kk[:, :], f[:, :], -1.0, 1.0, ALU.mult, ALU.add, reads=[f_r], writes=[kk_r], eng="pool")
            B.ts(f[:, :], f[:, :], 1e-20, None, ALU.max, reads=[f_r], writes=[f_r])
            B.act(f[:, :], f[:, :], AF.Ln, reads=[f_r], writes=[f_r])
            B.s.op("dve", lambda e, o=bb[:, :], d0=cst["smask"][:, 0:n_tok], d1=f[:, :]: e.tensor_tensor_scan(
                out=o, data0=d0, data1=d1, initial=0.0, op0=ALU.mult, op1=ALU.add), reads=[f_r, c_r], writes=[bb_r])
            b3 = bb[:, :].rearrange("p (c j) -> p c j", j=64)
            bmid = b3[:, :, 31:32].broadcast_to([128, NC, 64])
            blast = b3[:, :, 63:64].broadcast_to([128, NC, 64])
            e13 = e1[:, :].rearrange("p (c j) -> p c j", j=64)
            B.tt(e13, b3, bmid, ALU.subtract, reads=[bb_r], writes=[e1_r])
            B.act(e2[:, :], e1[:, :], AF.Exp, reads=[e1_r], writes=[e2_r])
            B.tt(d["qt"][:, :], q[:, :], e2[:, :], ALU.mult, reads=[q_r, e2_r], writes=[d["qt_r"]])
            B.act(e2[:, :], e1[:, :], AF.Exp, reads=[e1_r], writes=[e2_r], scale=-1.0)
            B.tt(d["kt"][:, :], kk[:, :], e2[:, :], ALU.mult, reads=[kk_r, e2_r], writes=[d["kt_r"]], eng="pool")
            B.act(e2[:, :], bb[:, :], AF.Exp, reads=[bb_r], writes=[e2_r])
            B.tt(d["qh"][:, :], q[:, :], e2[:, :], ALU.mult, reads=[q_r, e2_r], writes=[d["qh_r"]])
            B.copy(d["ebl"][:, :], e2[:, :].rearrange("p (c j) -> p c j", j=64)[:, :, 63], reads=[e2_r], writes=[d["ebl_r"]], eng="dve")
            B.tt(e13, blast, b3, ALU.subtract, reads=[bb_r], writes=[e1_r])
            B.act(e2[:, :], e1[:, :], AF.Exp, reads=[e1_r], writes=[e2_r])
            B.tt(d["kh"][:, :], kk[:, :], e2[:, :], ALU.mult, reads=[kk_r, e2_r], writes=[d["kh_r"]], eng="pool")

        def chain(h, d):
            for c in range(NC):
                cs = slice(c * 64, (c + 1) * 64)
                B.mm(d["psS"][0:64, 0:64], d["kt"][:, cs], d["qt"][:, cs], True, True, reads=[d["kt_r"], d["qt_r"]], writes=[d["psS_r"]])
                if c < NC - 1:
                    B.tr(d["tp"][0:64, 0, :], d["kh"][:, cs], ident[:, :], reads=[d["kh_r"]], writes=[d["tp_r"]])
                yield
                B.tt(d["ATm"][:, :], d["psS"][0:64, 0:64], cst["causT"][:, :], ALU.mult, reads=[d["psS_r"], c_r], writes=[d["ATm_r"]])
                if c < NC - 1:
                    B.copy(d["khT"][:, :], d["tp"][0:64, 0, :], reads=[d["tp_r"]], writes=[d["khT_r"]], eng="act")
                yield
                B.mm(d["psO"][:, 0:64], d["vv"][:, c, :], d["ATm"][:, :], True, c == 0, reads=[d["vv_r"], d["ATm_r"]], writes=[d["psO_r"]])
                if c > 0:
                    B.mm(d["psO"][:, 0:64], d["Sb"][:, :], d["qh"][:, cs], False, True, reads=[d["Sb_r"], d["qh_r"]], writes=[d["psO_r"]])
                if c < NC - 1:
                    B.mm(d["psU"][:, 0:128], d["khT"][:, :], d["vv"][:, c, :], True, True, reads=[d["khT_r"], d["vv_r"]], writes=[d["psU_r"]])
                yield
                B.copy(d["oall"][:, cs], d["psO"][:, 0:64], reads=[d["psO_r"]], writes=[d["oall_r"]], eng="act")
                if c < NC - 1:
                    if c == 0:
                        B.copy(d["St"][:, :], d["psU"][:, 0:128], reads=[d["psU_r"]], writes=[d["St_r"]], eng="dve")
                    else:
                        B.stt(d["St"][:, :], d["St"][:, :], d["ebl"][:, c:c + 1], d["psU"][:, 0:128], ALU.mult, ALU.add,
                              reads=[d["psU_r"], d["St_r"], d["ebl_r"]], writes=[d["St_r"]])
                    B.copy(d["Sb"][:, :], d["St"][:, :], reads=[d["St_r"]], writes=[d["Sb_r"]], eng="dve")
                yield

        for pair in range(HH // 2):
            hs = [2 * pair, 2 * pair + 1]
            for j, h in enumerate(hs):
                prep(h, hb[j])
            round_robin([chain(h, hb[j]) for j, h in enumerate(hs)])
            for j, h in enumerate(hs):
                d = hb[j]
                nb = (d["psS"], d["psS_r"], d["psO"], d["psO_r"], None, Res(), nbs[0], nbr[0], nbs[1], nbr[1], nbs[2], nbr[2])
                fm_norm(B, d["oall"], d["oall_r"], n_tok, "rms", hw[:, h:h + 1], hw_r, d["gT"], d["gT_r"], scr["CAT"][12 + h], cst, nb)
        s.barrier()
        B.stack = old


def load_wo(B, w_o, wo, wo_r):
    wv = w_o.rearrange("(k p) f -> p k f", p=128)
    for k in range(KC):
        B.dma("pool", wo[:, k, :], wv[:, k, :], writes=[wo_r[k]])


def oproj_stage(B, h_dram, h_res, wo, wo_r, scr, n_tok=S):
    s = B.s
    NT = n_tok // 128
    TBk = min(4, NT)
    with ExitStack() as sub:
        B.stack, old = sub, B.stack
        cat = [B.sb([128, KC, TBk * 128], BF16) for _ in range(2)]; cat_r = rl(2, "cat")
        ht = [B.sb([128, D], F32) for _ in range(3)]; ht_r = rl(3, "ht")
        po = [B.ps([128, 512], F32) for _ in range(8)]; po_r = rl(8, "po")
        for c in range(NT):
            cb, cc = c // TBk, c % TBk
            b = cb % 2
            hb_ = c % 3
            if cc == 0:
                B.dma("sp", cat[b][:, :, :], scr["CAT"][:, :, cb * TBk * 128:(cb + 1) * TBk * 128].rearrange("k p t -> p k t"), writes=[cat_r[b]])
            B.dma("sp", ht[hb_][:, :], h_dram[c * 128:(c + 1) * 128, :], reads=[h_res[c]], writes=[ht_r[hb_]])
            for nbk in range(4):
                pi = (c % 2) * 4 + nbk
                for k in range(KC):
                    B.mm(po[pi][:, :], cat[b][:, k, cc * 128:(cc + 1) * 128], wo[:, k, nbk * 512:(nbk + 1) * 512], k == 0, k == KC - 1,
                         reads=[cat_r[b], wo_r[k]], writes=[po_r[pi]])
                hs = ht[hb_][:, nbk * 512:(nbk + 1) * 512]
                B.tt(hs, hs, po[pi][:, :], ALU.add, reads=[po_r[pi], ht_r[hb_]], writes=[ht_r[hb_]])
            B.dma("sp", h_dram[c * 128:(c + 1) * 128, :], ht[hb_][:, :], reads=[ht_r[hb_]], writes=[h_res[c]])
        s.barrier()
        B.stack = old


def final_stage(B, h_dram, h_res, fnw, n_tok=S):
    s = B.s
    NT = n_tok // 128
    with ExitStack() as sub:
        B.stack, old = sub, B.stack
        fw = B.sb([128, D], F32); fw_r = Res()
        B.dma("sp", fw[:, :], fnw.partition_broadcast(128), writes=[fw_r])
        ht = [B.sb([128, D], F32) for _ in range(2)]; ht_r = rl(2, "ht")
        sq = B.sb([128, D], BF16); sq_r = Res()
        ss = [B.sb([128, 4], F32) for _ in range(2)]; ss_r = rl(2, "ss")
        eps = B.sb([128, 1], F32); eps_r = Res()
        B.memset(eps[:, :], EPS, writes=[eps_r])
        for c in range(NT):
            b = c % 2
            B.dma("sp", ht[b][:, :], h_dram[c * 128:(c + 1) * 128, :], reads=[h_res[c]], writes=[ht_r[b]])
            B.act(sq[:, :], ht[b][:, :], AF.Square, reads=[ht_r[b]], writes=[sq_r, ss_r[b]], accum_out=ss[b][:, 0:1])
            B.act(ss[b][:, 1:2], ss[b][:, 0:1], AF.Sqrt, reads=[ss_r[b], eps_r], writes=[ss_r[b]], scale=1.0 / D, bias=eps[:, 0:1])
            B.recip(ss[b][:, 2:3], ss[b][:, 1:2], reads=[ss_r[b]], writes=[ss_r[b]])
            B.stt(ht[b][:, :], ht[b][:, :], ss[b][:, 2:3], fw[:, :], ALU.mult, ALU.mult, reads=[ht_r[b], ss_r[b], fw_r], writes=[ht_r[b]])
            B.dma("sp", h_dram[c * 128:(c + 1) * 128, :], ht[b][:, :], reads=[ht_r[b]], writes=[h_res[c]])
        s.barrier()
        B.stack = old


def lb_compute(B, lg_dram, lb, lb_r):
    with ExitStack() as sub:
        B.stack, old = sub, B.stack
        lg = B.sb([128, 4, 4], F32); r = Res()
        m = B.sb([128, 4], F32)
        B.dma("sp", lg[:, :, :], lg_dram.rearrange("l (h p) -> p l h", p=128), writes=[r], allow_slow_non_contiguous=True)
        B.tt(m[:, :], lg[:, 0, :], lg[:, 1, :], ALU.max, reads=[r], writes=[r])
        B.tt(m[:, :], m[:, :], lg[:, 2, :], ALU.max, reads=[r], writes=[r])
        B.tt(m[:, :], m[:, :], lg[:, 3, :], ALU.max, reads=[r], writes=[r])
        for l in range(4):
            B.tt(lg[:, l, :], lg[:, l, :], m[:, :], ALU.subtract, reads=[r], writes=[r])
        B.act(lg[:, :, :], lg[:, :, :], AF.Exp, reads=[r], writes=[r])
        B.tt(m[:, :], lg[:, 0, :], lg[:, 1, :], ALU.add, reads=[r], writes=[r])
        B.tt(m[:, :], m[:, :], lg[:, 2, :], ALU.add, reads=[r], writes=[r])
        B.tt(m[:, :], m[:, :], lg[:, 3, :], ALU.add, reads=[r], writes=[r])
        B.recip(m[:, :], m[:, :], reads=[r], writes=[r])
        for l in range(4):
            B.tt(lg[:, l, :], lg[:, l, :], m[:, :], ALU.mult, reads=[r], writes=[r])
        B.memset(lb[:, 0, :], 0.0, writes=[lb_r])
        B.copy(lb[:, 1, :], lg[:, 1, :], reads=[r], writes=[lb_r])
        B.tt(lb[:, 2, :], lb[:, 1, :], lg[:, 2, :], ALU.add, reads=[r, lb_r], writes=[lb_r])
        B.tt(lb[:, 3, :], lb[:, 2, :], lg[:, 3, :], ALU.add, reads=[r, lb_r], writes=[lb_r])
        B.ts(lb[:, 4:8, :], lb[:, 0:4, :], -1.0, 1.0, ALU.mult, ALU.add, reads=[lb_r], writes=[lb_r])
        B.s.barrier()
        B.stack = old


def host_consts(n_tok=S):
    import ml_dtypes
    bf = ml_dtypes.bfloat16
    c = {}
    c["ident"] = np.eye(128, dtype=bf)
    qi = np.arange(128)
    c["maskneg"] = np.where(qi[None, :] <= qi[:, None], 0.0, -1e30).astype(bf)
    c["ones"] = np.ones((128, 128), np.float32)

    def rope(dim):
        inv = (10000.0 ** (-np.arange(0, dim, 2, dtype=np.float32) / np.float32(dim))).astype(np.float32)
        ang = (np.arange(n_tok, dtype=np.float32)[:, None] * inv[None, :]).astype(np.float32)
        co, si = np.cos(ang).astype(np.float32).T, np.sin(ang).astype(np.float32).T
        return np.concatenate([co, co], 0), np.concatenate([-si, si], 0)
    c["cosm"], c["sinm"] = rope(64)
    c["cosr"], c["sinr"] = rope(128)
    lg = np.log1p(-(2.0 ** (-5.0 - np.arange(4, dtype=np.float32)))).astype(np.float32)
    idx = np.arange(128, dtype=np.float32)
    rel = idx[None, :] - idx[:, None]
    dm = np.where(rel >= 0, np.exp(lg[:, None, None] * np.maximum(rel, 0.0)[None]), 0.0) * (128.0 ** -0.5)
    c["dmT"] = np.ascontiguousarray(dm.transpose(1, 0, 2)).astype(np.float32)
    xi = np.exp(lg[:, None] * (idx[None, :] + 1.0))
    c["xi"] = np.ascontiguousarray(np.broadcast_to(xi[None], (128, 4, 128))).astype(np.float32)
    zeta = np.exp(lg[:, None] * (127.0 - idx[None, :])) * (128.0 ** -0.5)
    c["zeta"] = np.ascontiguousarray(zeta.T).astype(np.float32)
    j64 = np.arange(64)
    c["causT"] = (j64[None, :] >= j64[:, None]).astype(np.float32)
    sm = np.ones((128, n_tok), np.float32)
    sm[:, ::64] = 0.0
    c["smask"] = sm
    return c


CONST_SPECS = [("ident", [128, 128], BF16), ("maskneg", [128, 128], BF16), ("ones", [128, 128], F32),
               ("dmT", [128, 4, 128], F32), ("xi", [128, 4, 128], F32), ("zeta", [128, 4], F32),
               ("causT", [64, 64], F32)]
TABLE_SPECS = ["cosm", "sinm", "cosr", "sinr", "smask"]
WEIGHT_SPECS = [("ffn1_norm", [DEPTH, D]), ("ffn1_w1", [DEPTH, D, DFF]), ("ffn1_w3", [DEPTH, D, DFF]),
                ("ffn1_w2", [DEPTH, DFF, D]), ("mix_norm", [DEPTH, D]), ("w_in", [DEPTH, D, D_IN]),
                ("mla_kv_norm", [DEPTH, 512]), ("mla_w_kv_b", [DEPTH, 512, 2048]), ("mla_out_norm", [DEPTH, 1024]),
                ("ret_gn", [DEPTH, 512]), ("hgrn_lb_logits", [DEPTH, 512]), ("hgrn_out_norm", [DEPTH, 512]),
                ("w_o", [DEPTH, D, D]), ("ffn2_norm", [DEPTH, D]), ("ffn2_w1", [DEPTH, D, DFF]),
                ("ffn2_w3", [DEPTH, D, DFF]), ("ffn2_w2", [DEPTH, DFF, D]), ("final_norm", [D])]


def build_program(n_tok=S, layers=range(DEPTH), stages=("ffn1", "proj", "mla", "ret", "hgrn", "oproj", "ffn2", "final"),
                  ffn_T=512, dump=None):
    nc = bass.Bass("TRN2", target_bir_lowering=False)
    x = nc.dram_tensor("x", [n_tok, D], F32, kind="ExternalInput").ap()
    W = {name: nc.dram_tensor(name, shp, F32, kind="ExternalInput").ap() for name, shp in WEIGHT_SPECS}
    Cd = {name: nc.dram_tensor(name, shp, dt, kind="ExternalInput").ap() for name, shp, dt in CONST_SPECS}
    for name in TABLE_SPECS:
        rows = 64 if name in ("cosm", "sinm") else 128
        Cd[name] = nc.dram_tensor(name, [rows, n_tok], F32, kind="ExternalInput").ap()
    out = nc.dram_tensor("out", [n_tok, D], F32, kind="ExternalOutput").ap()
    scr = {
        "QN": nc.dram_tensor("s_qn", [HM, 128, n_tok], BF16).ap(), "QR": nc.dram_tensor("s_qr", [HM, 64, n_tok], BF16).ap(),
        "KR": nc.dram_tensor("s_kr", [64, n_tok], BF16).ap(), "CKVN": nc.dram_tensor("s_ckvn", [4, 128, n_tok], BF16).ap(),
        "RQ": nc.dram_tensor("s_rq", [HR, 128, n_tok], BF16).ap(), "RK": nc.dram_tensor("s_rk", [HR, 128, n_tok], BF16).ap(),
        "RV": nc.dram_tensor("s_rv", [n_tok, 512], BF16).ap(), "RG": nc.dram_tensor("s_rg", [HR, 128, n_tok], BF16).ap(),
        "HQ": nc.dram_tensor("s_hq", [HH, 128, n_tok], F32).ap(), "HF": nc.dram_tensor("s_hf", [HH, 128, n_tok], F32).ap(),
        "HI": nc.dram_tensor("s_hi", [n_tok, 512], BF16).ap(), "HG": nc.dram_tensor("s_hg", [HH, 128, n_tok], BF16).ap(),
        "CAT": nc.dram_tensor("s_cat", [16, 128, n_tok], BF16).ap(),
    }
    if dump:
        dump_out = {k: nc.dram_tensor("d_" + k, list(scr[k].shape), scr[k].dtype, kind="ExternalOutput").ap() for k in dump}
    with ExitStack() as stack:
        B = Builder(nc, stack)
        s = B.s
        NT = n_tok // 128
        cst = {"c_r": Res("consts")}
        for name, shp, dt in CONST_SPECS:
            cst[name] = B.sb(shp, dt, "c_" + name)
            B.dma("sp", cst[name][tuple(slice(None) for _ in shp)], Cd[name])
        cst["eps"] = B.sb([128, 1], F32, "c_eps"); cst["eps_r"] = Res("eps")
        B.memset(cst["eps"][:, :], EPS)
        lb = B.sb([128, 8, 4], F32, "c_lb"); lb_r = Res("lb")
        h_res = rl(NT, "h")
        for i in range(NT):
            B.dma("sp", out[i * 128:(i + 1) * 128, :], x[i * 128:(i + 1) * 128, :], writes=[h_res[i]])
        s.barrier()
        lb_compute(B, W["hgrn_lb_logits"], lb, lb_r)

        def run_ffn(l, pre):
            with ExitStack() as sub:
                B.stack, old = sub, B.stack
                st = alloc_ffn_state(B, ffn_T, cst["ident"])
                ffn_stage(B, out, h_res, W[pre + "_norm"][l], W[pre + "_w1"][l], W[pre + "_w3"][l], W[pre + "_w2"][l],
                          ffn_T, st, n_tok=n_tok)
                s.barrier()
                B.stack = old

        for l in layers:
            if "ffn1" in stages:
                run_ffn(l, "ffn1")
            if "proj" in stages:
                with ExitStack() as sub:
                    B.stack, old = sub, B.stack
                    for name in ("cosm", "sinm", "cosr", "sinr"):
                        rows = 64 if name in ("cosm", "sinm") else 128
                        cst[name] = B.sb([rows, n_tok], F32)
                        B.dma("sp", cst[name][:, :], Cd[name])
                    s.barrier()
                    proj_stage(B, out, h_res, W["mix_norm"][l], W["w_in"][l], W["mla_kv_norm"][l], cst, scr, n_tok=n_tok)
                    B.stack = old
            if "mla" in stages:
                mla_stage(B, W["mla_w_kv_b"][l], W["mla_out_norm"][l], cst, scr, n_tok=n_tok)
            if "ret" in stages:
                ret_stage(B, W["ret_gn"][l], cst, scr, n_tok=n_tok)
            with ExitStack() as sub_o:
                B.stack, old_o = sub_o, B.stack
                if "oproj" in stages:
                    wo = B.sb([128, KC, D], BF16); wo_r = rl(KC, "wo")
                    load_wo(B, W["w_o"][l], wo, wo_r)
                if "hgrn" in stages:
                    with ExitStack() as sub:
                        B.stack, old = sub, B.stack
                        cst["smask"] = B.sb([128, n_tok], F32)
                        B.dma("sp", cst["smask"][:, :], Cd["smask"])
                        s.barrier()
                        hgrn_stage(B, lb, lb_r, l, W["hgrn_out_norm"][l], cst, scr, n_tok=n_tok)
                        B.stack = old
                if "oproj" in stages:
                    oproj_stage(B, out, h_res, wo, wo_r, scr, n_tok=n_tok)
                B.stack = old_o
            if "ffn2" in stages:
                run_ffn(l, "ffn2")
        if "final" in stages:
            final_stage(B, out, h_res, W["final_norm"], n_tok=n_tok)
        if dump:
            for k in dump:
                B.dma("sp", dump_out[k], scr[k])
        s.barrier()
        s.wait_all("sp", h_res)
        s.emit(nc, stack)
    return nc


_CONSTS = None


def kernel(**inputs):
    global _CONSTS
    nc = build_program()
    if _CONSTS is None:
        _CONSTS = host_consts(S)
    x = np.ascontiguousarray(inputs["x"], dtype=np.float32)
    shared = {name: np.ascontiguousarray(inputs[name], dtype=np.float32) for name, _ in WEIGHT_SPECS}
    shared.update(_CONSTS)
    in_maps = []
    for c in range(N_CORES):
        m = dict(shared)
        m["x"] = x[c]
        in_maps.append(m)
    res = run_bass_kernel_spmd(nc, in_maps, core_ids=list(range(N_CORES)))
    return np.stack([res.results[c]["out"] for c in range(N_CORES)], axis=0).astype(np.float32)
```

```python
from contextlib import ExitStack

import numpy as np
import concourse.bass as bass
import concourse.mybir as mybir
from concourse.bass_utils import run_bass_kernel_spmd

F32 = mybir.dt.float32
BF16 = mybir.dt.bfloat16
ALU = mybir.AluOpType
AF = mybir.ActivationFunctionType
AX = mybir.AxisListType

D = 2048
S = 2048
DEPTH = 4
DFF = 5632
NFF = DFF // 128
KC = D // 128
D_IN = 6208
EPS = 1e-6
N_CORES = 8
DEBUG_STOP = 3


class Tok:
    __slots__ = ("sem", "val", "eng")

    def __init__(self, sem, val, eng):
        self.sem, self.val, self.eng = sem, val, eng


class Res:
    __slots__ = ("name", "w", "r")

    def __init__(self, name=""):
        self.name = name
        self.w = None
        self.r = {}


class Sched:
    COMPUTE = ("pe", "dve", "act", "pool")
    ROT = 12000

    def __init__(self, n_dma_sems=24):
        self.streams = {e: [] for e in ("pe", "dve", "act", "pool", "sp")}
        self.n_sems = 0
        self.cur = {}
        for e in self.COMPUTE:
            self.cur[e] = [self._new_sem(), 0]
        self.dsem = {q: [[self._new_sem(), 0] for _ in range(n_dma_sems)] for q in ("sp", "pool", "act")}
        self.dma_rr = {q: 0 for q in self.dsem}
        self.waited = {e: {} for e in self.streams}
        self.pending = {e: [] for e in self.COMPUTE}
        self.n_ops = 0

    def _new_sem(self):
        self.n_sems += 1
        return self.n_sems - 1

    def _deps(self, reads, writes):
        deps = []
        for r in reads:
            if r.w is not None:
                deps.append(r.w)
        for w in writes:
            if w.w is not None:
                deps.append(w.w)
            deps.extend(w.r.values())
        return deps

    def _emit_waits(self, eng, deps):
        st = self.streams[eng]
        wd = self.waited[eng]
        for t in deps:
            if t.eng == "pe" and eng == "pe":
                continue
            assert t.val is not None, f"dependency on unsignalled op ({t.eng}->{eng})"
            if wd.get(t.sem, 0) < t.val:
                wd[t.sem] = t.val
                st.append(("wait", t.sem, t.val))

    def _mark(self, tok, reads, writes, key):
        for r in reads:
            r.r[key] = tok
        for w in writes:
            w.w = tok
            w.r = {}

    def op(self, eng, fn, reads=(), writes=(), signal=True):
        self.n_ops += 1
        self._emit_waits(eng, self._deps(reads, writes))
        tok = Tok(None, None, eng)
        if signal:
            c = self.cur[eng]
            if c[1] >= self.ROT:
                c[0], c[1] = self._new_sem(), 0
            c[1] += 1
            tok.sem, tok.val = c[0], c[1]
            for p in self.pending[eng]:
                p.sem, p.val = tok.sem, tok.val
            self.pending[eng] = []
            self.streams[eng].append(("op", fn, tok.sem, 1))
        else:
            self.pending[eng].append(tok)
            self.streams[eng].append(("op", fn, None, 0))
        self._mark(tok, reads, writes, ("e", eng))
        return tok

    def dma(self_, q, fn, reads=(), writes=()):
        self = self_
        self.n_ops += 1
        k = self.dma_rr[q]
        self.dma_rr[q] = (k + 1) % len(self.dsem[q])
        sem, cnt = self.dsem[q][k]
        deps = self._deps(reads, writes)
        if cnt > 0:
            deps.append(Tok(sem, cnt * 16, "dma"))
        self._emit_waits(q, deps)
        self.dsem[q][k][1] = cnt + 1
        tok = Tok(sem, (cnt + 1) * 16, "dma")
        self.streams[q].append(("op", fn, sem, 16))
        self._mark(tok, reads, writes, ("d", sem))
        return tok

    def wait_all(self, eng, resources):
        deps = []
        for r in resources:
            if r.w is not None:
                deps.append(r.w)
            deps.extend(r.r.values())
        self._emit_waits(eng, deps)

    def barrier(self):
        toks = []
        for e in self.COMPUTE:
            assert not self.pending[e], f"barrier with unsignalled ops on {e}"
            c = self.cur[e]
            if c[1] > 0:
                toks.append(Tok(c[0], c[1], e + "_b"))
        for q in self.dsem:
            for sem, cnt in self.dsem[q]:
                if cnt > 0:
                    toks.append(Tok(sem, cnt * 16, "dma"))
        for eng in self.streams:
            self._emit_waits(eng, toks)

    def emit(self, nc, stack):
        for e in self.COMPUTE:
            assert not self.pending[e], f"unsignalled trailing ops on {e}"
        sems = [stack.enter_context(nc.semaphore(f"s{i}")) for i in range(self.n_sems)]
        engs = {"pe": "tensor", "dve": "vector", "act": "scalar", "pool": "gpsimd", "sp": "sync"}
        block = stack.enter_context(nc.Block())
        for e, attr in engs.items():
            stream = self.streams[e]

            def body(engine, stream=stream):
                for item in stream:
                    if item[0] == "wait":
                        engine.wait_ge(sems[item[1]], item[2])
                    else:
                        ins = item[1](engine)
                        if item[2] is not None:
                            ins.then_inc(sems[item[2]], item[3])

            getattr(block, attr)(body)


class Builder:
    def __init__(self, nc, stack):
        self.nc = nc
        self.stack = stack
        self.s = Sched()
        self.uid = 0

    def sb(self, shape, dtype, name=None):
        self.uid += 1
        return self.stack.enter_context(self.nc.sbuf_tensor(name or f"sb{self.uid}", list(shape), dtype))

    def ps(self, shape, dtype, name=None):
        self.uid += 1
        return self.stack.enter_context(self.nc.psum_tensor(name or f"ps{self.uid}", list(shape), dtype))


    def dma(self, q, out, in_, reads=(), writes=(), **kw):
        return self.s.dma(q, lambda e: e.dma_start(out=out, in_=in_, **kw), reads, writes)

    def act(self, out, in_, func, reads=(), writes=(), **kw):
        return self.s.op("act", lambda e: e.activation(out=out, in_=in_, func=func, **kw), reads, writes)

    def mm(self, out, lhsT, rhs, start, stop, reads=(), writes=(), signal=None):
        return self.s.op("pe", lambda e: e.matmul(out, lhsT=lhsT, rhs=rhs, start=start, stop=stop), reads, writes,
                         signal=stop if signal is None else signal)

    def tr(self, out, in_, ident, reads=(), writes=(), signal=True):
        return self.s.op("pe", lambda e: e.transpose(out=out, in_=in_, identity=ident), reads, writes, signal=signal)

    def tt(self, out, in0, in1, op, reads=(), writes=(), eng="dve"):
        return self.s.op(eng, lambda e: e.tensor_tensor(out=out, in0=in0, in1=in1, op=op), reads, writes)

    def ts(self, out, in0, s1, s2, op0, op1=None, reads=(), writes=(), eng="dve", **kw):
        if op1 is None:
            return self.s.op(eng, lambda e: e.tensor_scalar(out=out, in0=in0, scalar1=s1, scalar2=None, op0=op0, **kw), reads, writes)
        return self.s.op(eng, lambda e: e.tensor_scalar(out=out, in0=in0, scalar1=s1, scalar2=s2, op0=op0, op1=op1, **kw), reads, writes)

    def stt(self, out, in0, scalar, in1, op0, op1, reads=(), writes=(), eng="dve"):
        return self.s.op(eng, lambda e: e.scalar_tensor_tensor(out=out, in0=in0, scalar=scalar, in1=in1, op0=op0, op1=op1),
                         reads, writes)

    def copy(self, out, in_, reads=(), writes=(), eng="dve"):
        if eng == "act":
            return self.s.op("act", lambda e: e.activation(out=out, in_=in_, func=AF.Copy), reads, writes)
        return self.s.op(eng, lambda e: e.tensor_copy(out=out, in_=in_), reads, writes)

    def recip(self, out, in_, reads=(), writes=()):
        return self.s.op("dve", lambda e: e.reciprocal(out=out, in_=in_), reads, writes)

    def memset(self, out, val, writes=(), eng="dve"):
        return self.s.op(eng, lambda e: e.memset(out, val), (), writes)


def ffn_stage(B, h_dram, h_res, norm_w, w1, w3, w2, T, st, n_tok=S):
    s = B.s
    TC = T // 128
    CB = st["CB"]
    NCB = DFF // CB
    FPB = CB // 128
    W2B = st["W2B"]
    NW2 = D // W2B
    ident = st["ident"]
    half = NFF // 2

    nw, nw_r = st["nw"], st["nw_r"]
    B.dma("sp", nw[:, :], norm_w.partition_broadcast(128), writes=[nw_r])

    w1v = w1.rearrange("(k p) f -> p k f", p=128)
    w3v = w3.rearrange("(k p) f -> p k f", p=128)
    w2v = w2.rearrange("(j p) d -> p j d", p=128)

    def load_w13(tile_idx, cb):
        slot = tile_idx % 2
        wt, wr = st["w13"][slot], st["w13_r"][slot]
        c0 = cb * CB
        B.dma("pool", wt[:, 0, :, :], w1v[:, :, c0:c0 + CB], writes=[wr[0]])
        B.dma("pool", wt[:, 1, :, :], w3v[:, :, c0:c0 + CB], writes=[wr[1]])

    def load_w2(tile_idx, nb):
        slot = tile_idx % 2
        wt, wr = st["w2"][slot], st["w2_r"][slot]
        c0 = nb * W2B
        B.dma("pool", wt[:, 0:half, :], w2v[:, 0:half, c0:c0 + W2B], writes=[wr[0]])
        B.dma("pool", wt[:, half:NFF, :], w2v[:, half:NFF, c0:c0 + W2B], writes=[wr[1]])

    n_tiles = n_tok // T
    w13_ctr = 0
    w2_ctr = 0
    ht, ht_r = st["ht"], st["ht_r"]
    xT, xT_r = st["xT"], st["xT_r"]
    gT, gT_r = st["gT"], st["gT_r"]
    hx, hx_r = st["hx"], st["hx_r"]

    def stats(src, src_r, c):
        sq, sq_r = st["sq"], st["sq_r"]
        ss, ss_r = st["ss"][c % 2], st["ss_r"][c % 2]
        xs, xs_r = st["xs"][c % 2], st["xs_r"][c % 2]
        B.act(sq[:, :], src, AF.Square, reads=[src_r], writes=[sq_r, ss_r], accum_out=ss[:, 0:1])
        B.act(ss[:, 1:2], ss[:, 0:1], AF.Sqrt, reads=[ss_r, st["eps_r"]], writes=[ss_r], scale=1.0 / D, bias=st["eps"][:, 0:1])
        B.recip(ss[:, 2:3], ss[:, 1:2], reads=[ss_r], writes=[ss_r])
        B.stt(xs[:, :], src, ss[:, 2:3], nw[:, :], ALU.mult, ALU.mult, reads=[src_r, ss_r, nw_r], writes=[xs_r])

    def transposes(c):
        xs, xs_r = st["xs"][c % 2], st["xs_r"][c % 2]
        for g in range(KC // 8):
            tp, tp_r = st["tp"][g % 2], st["tp_r"][g % 2]
            for k8 in range(8):
                k = g * 8 + k8
                B.tr(tp[:, k8, :], xs[:, k * 128:(k + 1) * 128], ident[:, :], reads=[xs_r], writes=[tp_r], signal=(k8 == 7))

    def evac(c):
        for g in range(KC // 8):
            tp, tp_r = st["tp"][g % 2], st["tp_r"][g % 2]
            B.copy(xT[:, g * 8:(g + 1) * 8, c * 128:(c + 1) * 128], tp[:, :, :], reads=[tp_r],
                   writes=xT_r[g * 8:(g + 1) * 8], eng="act" if g == 0 else "dve")

    for ti in range(n_tiles):
        t0 = ti * T
        for c in range(TC):
            B.dma("sp", ht[:, c, :], h_dram[t0 + c * 128:t0 + (c + 1) * 128, :], reads=[h_res[ti * TC + c]], writes=[ht_r[c]])
        if ti == 0:
            load_w13(w13_ctr, 0)
            stats(ht[:, 0, :], ht_r[0], 0)
            for c in range(TC):
                transposes(c)
                if c + 1 < TC:
                    stats(ht[:, c + 1, :], ht_r[c + 1], c + 1)
                evac(c)
        for cb in range(NCB):
            if cb + 1 < NCB:
                load_w13(w13_ctr + 1, cb + 1)
            else:
                load_w2(w2_ctr, 0)
            slot = w13_ctr % 2
            wt, wr = st["w13"][slot], st["w13_r"][slot]
            for f in range(FPB):
                j = cb * FPB + f
                pa, pa_r = st["pa"][j % 2], st["pa_r"][j % 2]
                pb, pb_r = st["pb"][j % 2], st["pb_r"][j % 2]
                sg, sg_r = st["sg"][j % 2], st["sg_r"][j % 2]
                for (which, pp, pp_r) in ((0, pa, pa_r), (1, pb, pb_r)):
                    for k in range(KC):
                        B.mm(pp[:, 0:T], wt[:, which, k, f * 128:(f + 1) * 128], xT[:, k, 0:T], k == 0, k == KC - 1,
                             reads=[wr[which], xT_r[k]], writes=[pp_r])
                B.act(sg[:, 0:T], pa[:, 0:T], AF.Silu, reads=[pa_r], writes=[sg_r])
                B.tt(gT[:, j, 0:T], sg[:, 0:T], pb[:, 0:T], ALU.mult, reads=[pb_r, sg_r], writes=[gT_r[j]])
            w13_ctr += 1
        pre = ti + 1 < n_tiles and NW2 >= 2 * TC
        for nb in range(NW2):
            if nb + 1 < NW2:
                load_w2(w2_ctr + 1, nb + 1)
            if pre:
                if nb == 0:
                    load_w13(w13_ctr, 0)
                c, odd = nb // 2, nb % 2
                if c < TC and not odd:
                    r0 = t0 + T + c * 128
                    B.dma("sp", hx[:, :], h_dram[r0:r0 + 128, :], reads=[h_res[(ti + 1) * TC + c]], writes=[hx_r])
                    stats(hx[:, :], hx_r, c)
                elif c < TC:
                    transposes(c)
                    evac(c)
            slot = w2_ctr % 2
            wt, wr = st["w2"][slot], st["w2_r"][slot]
            for c in range(TC):
                po, po_r = st["po"][(nb * TC + c) % 2], st["po_r"][(nb * TC + c) % 2]
                for j in range(NFF):
                    B.mm(po[:, 0:W2B], gT[:, j, c * 128:(c + 1) * 128], wt[:, j, :], j == 0, j == NFF - 1,
                         reads=[gT_r[j], wr[0 if j < half else 1]], writes=[po_r])
                hs = ht[:, c, nb * W2B:(nb + 1) * W2B]
                B.stt(hs, po[:, 0:W2B], 0.5, hs, ALU.mult, ALU.add, reads=[po_r, ht_r[c]], writes=[ht_r[c]])
            w2_ctr += 1
        for c in range(TC):
            B.dma("sp", h_dram[t0 + c * 128:t0 + (c + 1) * 128, :], ht[:, c, :], reads=[ht_r[c]], writes=[h_res[ti * TC + c]])


def alloc_ffn_state(B, T, ident):
    CB, W2B = 256, 256
    TC = T // 128
    st = {"CB": CB, "W2B": W2B, "ident": ident}

    def rl(n, name):
        return [Res(f"{name}{i}") for i in range(n)]

    st["nw"] = B.sb([128, D], F32); st["nw_r"] = Res("nw")
    st["eps"] = B.sb([128, 1], F32)
    st["eps_r"] = Res("eps")
    B.s.op("dve", lambda e: e.memset(st["eps"][:, :], EPS), writes=[st["eps_r"]])
    st["ht"] = B.sb([128, TC, D], F32); st["ht_r"] = rl(TC, "ht")
    st["hx"] = B.sb([128, D], F32); st["hx_r"] = Res("hx")
    st["sq"] = B.sb([128, D], BF16); st["sq_r"] = Res("sq")
    st["ss"] = [B.sb([128, 4], F32) for _ in range(2)]; st["ss_r"] = rl(2, "ss")
    st["xs"] = [B.sb([128, D], BF16) for _ in range(2)]; st["xs_r"] = rl(2, "xs")
    st["xT"] = B.sb([128, KC, T], BF16); st["xT_r"] = rl(KC, "xT")
    st["gT"] = B.sb([128, NFF, T], BF16); st["gT_r"] = rl(NFF, "gT")
    st["w13"] = [B.sb([128, 2, KC, CB], BF16) for _ in range(2)]; st["w13_r"] = [rl(2, f"w13_{i}") for i in range(2)]
    st["w2"] = [B.sb([128, NFF, W2B], BF16) for _ in range(2)]; st["w2_r"] = [rl(2, f"w2_{i}") for i in range(2)]
    st["sg"] = [B.sb([128, T], F32) for _ in range(2)]; st["sg_r"] = rl(2, "sg")
    st["tp"] = [B.ps([128, 8, 128], BF16) for _ in range(2)]; st["tp_r"] = rl(2, "tp")
    st["pa"] = [B.ps([128, 512], F32) for _ in range(2)]; st["pa_r"] = rl(2, "pa")
    st["pb"] = [B.ps([128, 512], F32) for _ in range(2)]; st["pb_r"] = rl(2, "pb")
    st["po"] = [B.ps([128, 512], F32) for _ in range(2)]; st["po_r"] = rl(2, "po")
    return st


HM, HR, HH = 8, 4, 4
C_MQ, C_CKV, C_KR, C_RQ, C_RK, C_RV, C_RG, C_HQ, C_HF, C_HI, C_HG = (
    0, 1536, 2048, 2112, 2624, 3136, 3648, 4160, 4672, 5184, 5696)
MLA_SCALE = 192.0 ** -0.5


def rl(n, name):
    return [Res(f"{name}{i}") for i in range(n)]


def round_robin(gens):
    gens = list(gens)
    while gens:
        for g in list(gens):
            try:
                next(g)
            except StopIteration:
                gens.remove(g)


def proj_stage(B, h_dram, h_res, norm_w, w_in, kvn_w, cst, scr, n_tok=S):
    s = B.s
    NT = n_tok // 128
    NB = max(1, n_tok // 512)
    TB = min(512, n_tok)
    with ExitStack() as sub:
        B.stack, old = sub, B.stack
        ident = cst["ident"]
        nw = B.sb([128, D], F32); nw_r = Res()
        kw = B.sb([128, 4], F32); kw_r = Res()
        B.dma("sp", nw[:, :], norm_w.partition_broadcast(128), writes=[nw_r])
        B.dma("sp", kw[:, :], kvn_w.rearrange("(k p) -> p k", p=128), writes=[kw_r], allow_slow_non_contiguous=True)
        xT = B.sb([128, KC, n_tok], BF16); xT_r = rl(KC, "xT")
        ht = [B.sb([128, D], F32) for _ in range(3)]; ht_r = rl(3, "ht")
        sq = B.sb([128, D], BF16); sq_r = Res()
        ss = [B.sb([128, 4], F32) for _ in range(2)]; ss_r = rl(2, "ss")
        xs = [B.sb([128, D], BF16) for _ in range(2)]; xs_r = rl(2, "xs")
        tp = [B.ps([128, 8, 128], BF16) for _ in range(2)]; tp_r = rl(2, "tp")
        pp = [B.ps([128, 512], F32) for _ in range(4)]; pp_r = rl(4, "pp")
        wb = [B.sb([128, KC, 512], BF16) for _ in range(2)]; wb_r = [rl(4, f"wb{i}_") for i in range(2)]
        ws = [B.sb([128, KC, 128], BF16) for _ in range(2)]; ws_r = [rl(2, f"ws{i}_") for i in range(2)]
        tabs = {"m": (cst["cosm"], cst["sinm"]), "r": (cst["cosr"], cst["sinr"])}
        stg = [B.sb([128, 512], BF16) for _ in range(3)]; stg_r = rl(3, "stg")
        stf = [B.sb([128, 512], F32) for _ in range(2)]; stf_r = rl(2, "stf")
        t1 = [B.sb([128, 512], F32) for _ in range(2)]; t1_r = rl(2, "t1")
        t2 = [B.sb([128, 512], F32) for _ in range(2)]; t2_r = rl(2, "t2")
        eps, eps_r = cst["eps"], cst["eps_r"]

        for c in range(min(2, NT)):
            B.dma("sp", ht[c % 3][:, :], h_dram[c * 128:(c + 1) * 128, :], reads=[h_res[c]], writes=[ht_r[c % 3]])

        def stats(c):
            b, h4 = c % 2, c % 3
            if c + 2 < NT:
                B.dma("sp", ht[(c + 2) % 3][:, :], h_dram[(c + 2) * 128:(c + 3) * 128, :], reads=[h_res[c + 2]], writes=[ht_r[(c + 2) % 3]])
            B.act(sq[:, :], ht[h4][:, :], AF.Square, reads=[ht_r[h4]], writes=[sq_r, ss_r[b]], accum_out=ss[b][:, 0:1])
            B.act(ss[b][:, 1:2], ss[b][:, 0:1], AF.Sqrt, reads=[ss_r[b], eps_r], writes=[ss_r[b]], scale=1.0 / D, bias=eps[:, 0:1])
            B.recip(ss[b][:, 2:3], ss[b][:, 1:2], reads=[ss_r[b]], writes=[ss_r[b]])
            B.stt(xs[b][:, :], ht[h4][:, :], ss[b][:, 2:3], nw[:, :], ALU.mult, ALU.mult, reads=[ht_r[h4], ss_r[b], nw_r], writes=[xs_r[b]])

        stats(0)
        for c in range(NT):
            b = c % 2
            for g in range(KC // 8):
                for k8 in range(8):
                    k = g * 8 + k8
                    B.tr(tp[g][:, k8, :], xs[b][:, k * 128:(k + 1) * 128], ident[:, :], reads=[xs_r[b]], writes=[tp_r[g]], signal=(k8 == 7))
            if c + 1 < NT:
                stats(c + 1)
            for g in range(KC // 8):
                B.copy(xT[:, g * 8:(g + 1) * 8, c * 128:(c + 1) * 128], tp[g][:, :, :], reads=[tp_r[g]],
                       writes=xT_r[g * 8:(g + 1) * 8], eng="act" if g == 0 else "dve")

        wv = w_in.rearrange("(k p) f -> p k f", p=128)
        ctr = {"wb": 0, "ws": 0, "pp": 0, "stg": 0, "stf": 0, "t": 0}

        def load_block(c0, ncols):
            i = ctr["wb"] % 2; ctr["wb"] += 1
            nq = (ncols + 127) // 128
            for q in range(nq):
                w_ = min(128, ncols - q * 128)
                B.dma("pool", wb[i][:, :, q * 128:q * 128 + w_], wv[:, :, c0 + q * 128:c0 + q * 128 + w_], writes=[wb_r[i][q]])
            return i

        def load_swapped(c0, n):
            i = ctr["ws"] % 2; ctr["ws"] += 1
            hf = n // 2
            B.dma("pool", ws[i][:, :, 0:hf], wv[:, :, c0 + hf:c0 + n], writes=[ws_r[i][0]])
            B.dma("pool", ws[i][:, :, hf:n], wv[:, :, c0:c0 + hf], writes=[ws_r[i][1]])
            return i

        def mm_fm(psum, psum_r, wt, wt_rs, M, tb):
            for k in range(KC):
                B.mm(psum[0:M, 0:TB], wt[:, k, :], xT[:, k, tb * TB:(tb + 1) * TB], k == 0, k == KC - 1,
                     reads=list(wt_rs) + [xT_r[k]], writes=[psum_r])

        def next_pp():
            i = ctr["pp"] % 4; ctr["pp"] += 1
            return pp[i], pp_r[i]

        def fm_tile(bi, q, M, kind, dst, tab=None, swi=None):
            wt = wb[bi][:, :, q * 128:q * 128 + M]
            for tb in range(NB):
                pa, pa_r = next_pp()
                mm_fm(pa, pa_r, wt, [wb_r[bi][q]], M, tb)
                dsl = dst[0:M, tb * TB:(tb + 1) * TB]
                if kind == "rope":
                    pb, pb_r = next_pp()
                    mm_fm(pb, pb_r, ws[swi][:, :, 0:M], ws_r[swi], M, tb)
                    ct, sn = tabs[tab]
                    j = ctr["t"] % 2; ctr["t"] += 1
                    B.tt(t1[j][0:M, 0:TB], pa[0:M, 0:TB], ct[0:M, tb * TB:(tb + 1) * TB], ALU.mult, reads=[pa_r], writes=[t1_r[j]])
                    B.tt(t2[j][0:M, 0:TB], pb[0:M, 0:TB], sn[0:M, tb * TB:(tb + 1) * TB], ALU.mult, reads=[pb_r], writes=[t2_r[j]])
                    g = ctr["stg"] % 3; ctr["stg"] += 1
                    B.tt(stg[g][0:M, 0:TB], t1[j][0:M, 0:TB], t2[j][0:M, 0:TB], ALU.add, reads=[t1_r[j], t2_r[j]], writes=[stg_r[g]])
                    B.dma("sp", dsl, stg[g][0:M, 0:TB], reads=[stg_r[g]])
                elif kind == "f32":
                    g = ctr["stf"] % 2; ctr["stf"] += 1
                    B.copy(stf[g][0:M, 0:TB], pa[0:M, 0:TB], reads=[pa_r], writes=[stf_r[g]], eng="dve")
                    B.dma("sp", dsl, stf[g][0:M, 0:TB], reads=[stf_r[g]])
                else:
                    g = ctr["stg"] % 3; ctr["stg"] += 1
                    B.act(stg[g][0:M, 0:TB], pa[0:M, 0:TB], AF.Silu if kind == "silu" else AF.Copy, reads=[pa_r], writes=[stg_r[g]])
                    B.dma("sp", dsl, stg[g][0:M, 0:TB], reads=[stg_r[g]])

        for h in range(HM):
            c0 = C_MQ + h * 192
            bi = load_block(c0, 192)
            swi = load_swapped(c0 + 128, 64)
            fm_tile(bi, 0, 128, "copy", scr["QN"][h])
            fm_tile(bi, 1, 64, "rope", scr["QR"][h], tab="m", swi=swi)
        bi = load_block(C_KR, 64)
        swi = load_swapped(C_KR, 64)
        fm_tile(bi, 0, 64, "rope", scr["KR"], tab="m", swi=swi)
        for (c0, dst) in ((C_RQ, scr["RQ"]), (C_RK, scr["RK"])):
            bi = load_block(c0, 512)
            for h in range(HR):
                swi = load_swapped(c0 + h * 128, 128)
                fm_tile(bi, h, 128, "rope", dst[h], tab="r", swi=swi)
        for (c0, dst, kind) in ((C_RG, scr["RG"], "silu"), (C_HQ, scr["HQ"], "f32"), (C_HF, scr["HF"], "f32"),
                                (C_HG, scr["HG"], "silu")):
            bi = load_block(c0, 512)
            for h in range(4):
                fm_tile(bi, h, 128, kind, dst[h])
        for (c0, dst) in ((C_RV, scr["RV"]), (C_HI, scr["HI"])):
            bi = load_block(c0, 512)
            for c in range(NT):
                pa, pa_r = next_pp()
                for k in range(KC):
                    B.mm(pa[:, 0:512], xT[:, k, c * 128:(c + 1) * 128], wb[bi][:, k, :], k == 0, k == KC - 1,
                         reads=wb_r[bi] + [xT_r[k]], writes=[pa_r])
                g = ctr["stg"] % 3; ctr["stg"] += 1
                B.copy(stg[g][:, :], pa[:, 0:512], reads=[pa_r], writes=[stg_r[g]], eng="act" if c % 2 else "dve")
                B.dma("sp", dst[c * 128:(c + 1) * 128, :], stg[g][:, :], reads=[stg_r[g]])
        bi = load_block(C_CKV, 512)
        for c in range(NT):
            b = c % 2
            pa, pa_r = next_pp()
            for k in range(KC):
                B.mm(pa[:, 0:512], xT[:, k, c * 128:(c + 1) * 128], wb[bi][:, k, :], k == 0, k == KC - 1,
                     reads=wb_r[bi] + [xT_r[k]], writes=[pa_r])
            B.act(sq[:, 0:512], pa[:, 0:512], AF.Square, reads=[pa_r], writes=[sq_r, ss_r[b]], accum_out=ss[b][:, 0:1])
            B.act(ss[b][:, 1:2], ss[b][:, 0:1], AF.Sqrt, reads=[ss_r[b], eps_r], writes=[ss_r[b]], scale=1.0 / 512, bias=eps[:, 0:1])
            B.recip(ss[b][:, 2:3], ss[b][:, 1:2], reads=[ss_r[b]], writes=[ss_r[b]])
            B.ts(xs[b][:, 0:512], pa[:, 0:512], ss[b][:, 2:3], None, ALU.mult, reads=[pa_r, ss_r[b]], writes=[xs_r[b]])
            g2 = c % 2
            for k4 in range(4):
                B.tr(tp[g2][:, k4, :], xs[b][:, k4 * 128:(k4 + 1) * 128], ident[:, :], reads=[xs_r[b]], writes=[tp_r[g2]], signal=(k4 == 3))
            g = ctr["stg"] % 3; ctr["stg"] += 1
            for k4 in range(4):
                B.act(stg[g][:, k4 * 128:(k4 + 1) * 128], tp[g2][:, k4, :], AF.Copy, reads=[tp_r[g2], kw_r], writes=[stg_r[g]], scale=kw[:, k4:k4 + 1])
            B.dma("sp", scr["CKVN"][:, :, c * 128:(c + 1) * 128].rearrange("k p t -> p k t"),
                  stg[g][:, :].rearrange("p (k t) -> p k t", k=4), reads=[stg_r[g]])
        s.barrier()
        B.stack = old


def mla_stage(B, wkvb, onw, cst, scr, n_tok=S):
    s = B.s
    NT = n_tok // 128
    with ExitStack() as sub:
        B.stack, old = sub, B.stack
        ident = cst["ident"]
        eps, eps_r = cst["eps"], cst["eps_r"]
        maskneg = cst["maskneg"]
        c_r = cst["c_r"]
        ckv = B.sb([128, 4, n_tok], BF16); ckv_r = Res()
        krT = B.sb([64, n_tok], BF16); kr_r = Res()
        B.dma("sp", ckv[:, :, :], scr["CKVN"][:, :, 0:n_tok].rearrange("k p t -> p k t"), writes=[ckv_r])
        B.dma("sp", krT[:, :], scr["KR"][:, 0:n_tok], writes=[kr_r])
        ow = B.sb([128, HM], F32); ow_r = Res()
        B.dma("sp", ow[:, :], onw.rearrange("(h p) -> p h", p=128), writes=[ow_r], allow_slow_non_contiguous=True)
        wk = [B.sb([128, 4, 256], BF16) for _ in range(2)]; wk_r = rl(2, "wk")
        qn = [B.sb([128, n_tok], BF16) for _ in range(2)]; qn_r = rl(2, "qn")
        qr = [B.sb([64, n_tok], BF16) for _ in range(2)]; qr_r = rl(2, "qr")
        kn = [B.sb([128, n_tok], BF16) for _ in range(2)]; kn_r = rl(2, "kn")
        vv = [B.sb([128, NT, 128], BF16) for _ in range(2)]; vv_r = rl(2, "vv")
        oT = [B.sb([128, n_tok], BF16) for _ in range(2)]; oT_r = rl(2, "oT")
        pr = [B.sb([128, n_tok], BF16) for _ in range(2)]; pr_r = rl(2, "pr")
        pT = [B.sb([128, NT, 128], BF16) for _ in range(2)]; pT_r = rl(2, "pT")
        mx = [B.sb([128, 2], F32) for _ in range(2)]; mx_r = rl(2, "mx")
        oacc = [B.sb([128, NT, 128], F32) for _ in range(2)]; oacc_r = rl(2, "oacc")
        ssum = [B.sb([128, NT, 1], F32) for _ in range(2)]; ssum_r = rl(2, "ssum")
        nst = B.sb([128, 3, NT], F32); nst_r = Res()
        sqb = B.sb([128, NT, 128], F32); sqb_r = Res()
        o2 = B.sb([128, NT, 128], BF16); o2_r = Res()
        scA = B.ps([128, 2048], F32); scB = B.ps([128, 1024], F32)
        sc = [scA, scB]; sc_rb = [rl(4, "scA"), rl(2, "scB")]
        scb16 = [scA[:, :].bitcast(BF16), scB[:, :].bitcast(BF16)]
        pX = B.ps([128, 512], F32); pY = B.ps([128, 512], F32)
        pxy = [pX, pY]; pxy_r = rl(2, "pxy")
        pYb = pY[:, :].bitcast(BF16)
        wv = wkvb.rearrange("(k p) f -> p k f", p=128)

        def head_prep(h):
            b = h % 2
            B.dma("pool", wk[b][:, :, :], wv[:, :, h * 256:(h + 1) * 256], writes=[wk_r[b]])
            B.dma("sp", qn[b][:, :], scr["QN"][h][:, 0:n_tok], writes=[qn_r[b]])
            B.dma("sp", qr[b][:, :], scr["QR"][h][:, 0:n_tok], writes=[qr_r[b]])
            for t2 in range(n_tok // 256):
                ts_ = slice(t2 * 256, (t2 + 1) * 256)
                for k in range(4):
                    B.mm(pX[:, 0:256], wk[b][:, k, 0:128], ckv[:, k, ts_], k == 0, k == 3, reads=[wk_r[b], ckv_r], writes=[pxy_r[0]])
                for cc in range(2):
                    c = t2 * 2 + cc
                    for k in range(4):
                        B.mm(pX[:, 256 + cc * 128:256 + (cc + 1) * 128], ckv[:, k, c * 128:(c + 1) * 128], wk[b][:, k, 128:256],
                             k == 0, k == 3, reads=[wk_r[b], ckv_r], writes=[pxy_r[0]], signal=(k == 3 and cc == 1))
                B.copy(kn[b][:, ts_], pX[:, 0:256], reads=[pxy_r[0]], writes=[kn_r[b]], eng="act")
                B.copy(vv[b][:, t2 * 2:t2 * 2 + 2, :], pX[:, 256:512].rearrange("p (c d) -> p c d", d=128),
                       reads=[pxy_r[0]], writes=[vv_r[b]], eng="act")

        order = []
        for a in range(NT // 2):
            order += [NT - 1 - a, a]
        steps = [(h, i) for h in range(HM) for i in order]

        def scores(t):
            h, i = steps[t]
            b, u = h % 2, t % 2
            q0 = i * 128
            for g0 in range(0, q0, 512):
                g1 = min(g0 + 512, q0)
                wr = [sc_rb[u][g0 // 512]]
                B.mm(sc[u][:, g0:g1], qn[b][:, q0:q0 + 128], kn[b][:, g0:g1], True, False, reads=[qn_r[b], kn_r[b]], writes=wr, signal=False)
                B.mm(sc[u][:, g0:g1], qr[b][:, q0:q0 + 128], krT[:, g0:g1], False, True, reads=[qr_r[b], kr_r], writes=wr, signal=False)
            wr = [sc_rb[u][q0 // 512]]
            B.mm(sc[u][:, q0:q0 + 128], qn[b][:, q0:q0 + 128], kn[b][:, q0:q0 + 128], True, False, reads=[qn_r[b], kn_r[b]], writes=wr, signal=False)
            B.mm(sc[u][:, q0:q0 + 128], qr[b][:, q0:q0 + 128], krT[:, q0:q0 + 128], False, False, reads=[qr_r[b], kr_r], writes=wr, signal=False)
            B.mm(sc[u][:, q0:q0 + 128], ident[:, :], maskneg[:, :], False, True, reads=[c_r], writes=wr, signal=True)

        def softmax(t):
            h, i = steps[t]
            b, u = h % 2, t % 2
            nk = (i + 1) * 128
            banks = sc_rb[u][0:(nk + 511) // 512]
            B.s.op("dve", lambda e, o=mx[u][:, 0:1], a=sc[u][:, 0:nk]: e.reduce_max(out=o, in_=a, axis=AX.X), reads=banks, writes=[mx_r[u]])
            B.ts(mx[u][:, 1:2], mx[u][:, 0:1], -MLA_SCALE, None, ALU.mult, reads=[mx_r[u]], writes=[mx_r[u]])
            B.act(pr[u][:, 0:nk], sc[u][:, 0:nk], AF.Exp, reads=banks + [mx_r[u]], writes=[pr_r[u], ssum_r[b]],
                  scale=MLA_SCALE, bias=mx[u][:, 1:2], accum_out=ssum[b][:, i, :])

        def pv(t):
            h, i = steps[t]
            b, u = h % 2, t % 2
            for c8 in range((i + 8) // 8):
                ncs = min(8, i + 1 - c8 * 8)
                for cc in range(ncs):
                    c = c8 * 8 + cc
                    B.tr(scb16[u][:, c * 128:(c + 1) * 128], pr[u][:, c * 128:(c + 1) * 128], ident[:, :], reads=[pr_r[u]],
                         writes=[sc_rb[u][c8]], signal=(cc == ncs - 1))
                B.copy(pT[u][:, c8 * 8:c8 * 8 + ncs, :], scb16[u][:, c8 * 1024:c8 * 1024 + ncs * 128].rearrange("p (c q) -> p c q", q=128),
                       reads=[sc_rb[u][c8]], writes=[pT_r[u]], eng="dve")
            for c in range(i + 1):
                B.mm(pxy[u][:, 0:128], pT[u][:, c, :], vv[b][:, c, :], c == 0, c == i, reads=[pT_r[u], vv_r[b]], writes=[pxy_r[u]])
            B.copy(oacc[b][:, i, :], pxy[u][:, 0:128], reads=[pxy_r[u]], writes=[oacc_r[b]], eng="dve")

        def head_norm(h):
            b = h % 2
            o3 = oacc[b][:, :, :]
            B.recip(nst[:, 0, :], ssum[b][:, :, :].rearrange("p n o -> p (n o)"), reads=[ssum_r[b]], writes=[nst_r])
            B.tt(o3, o3, nst[:, 0, :].rearrange("p (n o) -> p n o", o=1).broadcast_to([128, NT, 128]), ALU.mult, reads=[nst_r, oacc_r[b]], writes=[oacc_r[b]])
            B.tt(sqb[:, :, :], o3, o3, ALU.mult, reads=[oacc_r[b]], writes=[sqb_r])
            B.s.op("dve", lambda e, o=nst[:, 1, :], a=sqb[:, :, :]: e.tensor_reduce(out=o, in_=a, axis=AX.X, op=ALU.add), reads=[sqb_r], writes=[nst_r])
            B.act(nst[:, 2, :], nst[:, 1, :], AF.Sqrt, reads=[nst_r, eps_r], writes=[nst_r], scale=1.0 / 128, bias=eps[:, 0:1])
            B.recip(nst[:, 2, :], nst[:, 2, :], reads=[nst_r], writes=[nst_r])
            B.tt(o2[:, :, :], o3, nst[:, 2, :].rearrange("p (n o) -> p n o", o=1).broadcast_to([128, NT, 128]), ALU.mult, reads=[nst_r, oacc_r[b]], writes=[o2_r])
            for c8 in range((NT + 7) // 8):
                ncs = min(8, NT - c8 * 8)
                for cc in range(ncs):
                    i = c8 * 8 + cc
                    B.tr(pYb[:, cc * 128:(cc + 1) * 128], o2[:, i, :], ident[:, :], reads=[o2_r], writes=[pxy_r[1]], signal=(cc == ncs - 1))
                B.act(oT[b][:, c8 * 1024:c8 * 1024 + ncs * 128].rearrange("p (c q) -> p c q", q=128),
                      pYb[:, 0:ncs * 128].rearrange("p (c q) -> p c q", q=128), AF.Copy,
                      reads=[pxy_r[1], ow_r], writes=[oT_r[b]], scale=ow[:, h:h + 1])
            B.dma("sp", scr["CAT"][h][:, 0:n_tok], oT[b][:, :], reads=[oT_r[b]])

        head_prep(0)
        scores(0)
        softmax(0)
        nsteps = len(steps)
        for t in range(nsteps):
            h, i = steps[t]
            pos = t % NT
            if pos == min(2, NT - 1) and h + 1 < HM:
                head_prep(h + 1)
            if t + 1 < nsteps:
                scores(t + 1)
                softmax(t + 1)
            pv(t)
            if pos == NT - 1:
                head_norm(h)
        s.barrier()
        B.stack = old


def fm_norm(B, oall, oall_r, n_tok, mode, w_ap, w_r, gate, gate_r, dst, cst, bufs):
    TB = min(512, n_tok)
    ones = cst["ones"]
    eps, eps_r = cst["eps"], cst["eps_r"]
    ps1, ps1_r, ps2, ps2_r, dd, dd_r, sqb, sqb_r, rs, rs_r, og, og_r = bufs
    for tb in range(max(1, n_tok // 512)):
        sl = slice(tb * TB, (tb + 1) * TB)
        if mode == "gn":
            B.mm(ps1[:, 0:TB], ones[:, :], oall[:, sl], True, True, reads=[oall_r, cst["c_r"]], writes=[ps1_r])
            B.stt(dd[:, 0:TB], ps1[:, 0:TB], -1.0 / 128, oall[:, sl], ALU.mult, ALU.add, reads=[ps1_r, oall_r], writes=[dd_r])
            src = dd[:, 0:TB]
        else:
            src = oall[:, sl]
        B.tt(sqb[:, 0:TB], src, src, ALU.mult, reads=[dd_r, oall_r], writes=[sqb_r])
        B.mm(ps2[:, 0:TB], ones[:, :], sqb[:, 0:TB], True, True, reads=[sqb_r, cst["c_r"]], writes=[ps2_r])
        B.act(rs[:, 0:TB], ps2[:, 0:TB], AF.Sqrt, reads=[ps2_r, eps_r], writes=[rs_r], scale=1.0 / 128, bias=eps[:, 0:1])
        B.recip(rs[:, 0:TB], rs[:, 0:TB], reads=[rs_r], writes=[rs_r])
        B.tt(sqb[:, 0:TB], src, rs[:, 0:TB], ALU.mult, reads=[dd_r, oall_r, rs_r], writes=[sqb_r])
        B.stt(og[:, 0:TB], sqb[:, 0:TB], w_ap, gate[:, sl], ALU.mult, ALU.mult, reads=[sqb_r, w_r, gate_r], writes=[og_r])
        B.dma("sp", dst[:, sl], og[:, 0:TB], reads=[og_r])


def ret_stage(B, gnw, cst, scr, n_tok=S):
    s = B.s
    NT = n_tok // 128
    with ExitStack() as sub:
        B.stack, old = sub, B.stack
        ident = cst["ident"]
        c_r = cst["c_r"]
        gw = B.sb([128, HR], F32); gw_r = Res()
        B.dma("sp", gw[:, :], gnw.rearrange("(h p) -> p h", p=128), writes=[gw_r], allow_slow_non_contiguous=True)
        hb = []
        for j in range(2):
            d = {}
            d["qT"] = B.sb([128, n_tok], BF16); d["qT_r"] = Res()
            d["kT"] = B.sb([128, n_tok], BF16); d["kT_r"] = Res()
            d["gT"] = B.sb([128, n_tok], BF16); d["gT_r"] = Res()
            d["vv"] = B.sb([128, NT, 128], BF16); d["vv_r"] = Res()
            d["oall"] = B.sb([128, n_tok], F32); d["oall_r"] = Res()
            d["R"] = B.sb([128, 128], F32); d["R_r"] = Res()
            d["Rb"] = B.sb([128, 128], BF16); d["Rb_r"] = Res()
            d["AT"] = B.sb([128, 128], BF16); d["AT_r"] = Res()
            d["qx"] = B.sb([128, 128], BF16); d["qx_r"] = Res()
            d["kz"] = B.sb([128, 128], BF16); d["kz_r"] = Res()
            d["psS"] = B.ps([128, 512], F32); d["psS_r"] = Res()
            d["psO"] = B.ps([128, 512], F32); d["psO_r"] = Res()
            d["psU"] = B.ps([128, 512], F32); d["psU_r"] = Res()
            d["tp"] = B.ps([128, 8, 128], BF16); d["tp_r"] = Res()
            hb.append(d)
        nbs = B.sb([128, 512], F32), B.sb([128, 512], F32), B.sb([128, 512], F32), B.sb([128, 512], BF16)
        nbr = Res(), Res(), Res(), Res()

        def load(h, d):
            B.dma("sp", d["qT"][:, :], scr["RQ"][h][:, 0:n_tok], writes=[d["qT_r"]])
            B.dma("sp", d["kT"][:, :], scr["RK"][h][:, 0:n_tok], writes=[d["kT_r"]])
            B.dma("sp", d["gT"][:, :], scr["RG"][h][:, 0:n_tok], writes=[d["gT_r"]])
            B.dma("sp", d["vv"][:, :, :], scr["RV"][0:n_tok, h * 128:(h + 1) * 128].rearrange("(c p) e -> p c e", p=128), writes=[d["vv_r"]])

        def chain(h, d):
            gch = float(np.float32(np.exp(np.float32(np.log1p(-np.float32(2.0 ** (-5.0 - h)))) * np.float32(128.0))))
            qT, kT, vv = d["qT"], d["kT"], d["vv"]
            for n in range(NT):
                cs = slice(n * 128, (n + 1) * 128)
                B.mm(d["psS"][:, 0:128], kT[:, cs], qT[:, cs], True, True, reads=[d["kT_r"], d["qT_r"]], writes=[d["psS_r"]])
                if n < NT - 1:
                    B.tr(d["tp"][:, 0, :], kT[:, cs], ident[:, :], reads=[d["kT_r"]], writes=[d["tp_r"]])
                yield
                B.tt(d["AT"][:, :], d["psS"][:, 0:128], cst["dmT"][:, h, :], ALU.mult, reads=[d["psS_r"], c_r], writes=[d["AT_r"]])
                if n > 0:
                    B.tt(d["qx"][:, :], qT[:, cs], cst["xi"][:, h, :], ALU.mult, reads=[d["qT_r"], c_r], writes=[d["qx_r"]], eng="pool")
                if n < NT - 1:
                    B.act(d["kz"][:, :], d["tp"][:, 0, :], AF.Copy, reads=[d["tp_r"], c_r], writes=[d["kz_r"]], scale=cst["zeta"][:, h:h + 1])
                yield
                B.mm(d["psO"][:, 0:128], vv[:, n, :], d["AT"][:, :], True, n == 0, reads=[d["vv_r"], d["AT_r"]], writes=[d["psO_r"]])
                if n > 0:
                    B.mm(d["psO"][:, 0:128], d["Rb"][:, :], d["qx"][:, :], False, True, reads=[d["Rb_r"], d["qx_r"]], writes=[d["psO_r"]])
                if n < NT - 1:
                    B.mm(d["psU"][:, 0:128], d["kz"][:, :], vv[:, n, :], True, True, reads=[d["kz_r"], d["vv_r"]], writes=[d["psU_r"]])
                yield
                B.copy(d["oall"][:, cs], d["psO"][:, 0:128], reads=[d["psO_r"]], writes=[d["oall_r"]], eng="act")
                if n < NT - 1:
                    if n == 0:
                        B.copy(d["R"][:, :], d["psU"][:, 0:128], reads=[d["psU_r"]], writes=[d["R_r"]], eng="dve")
                    else:
                        B.stt(d["R"][:, :], d["R"][:, :], gch, d["psU"][:, 0:128], ALU.mult, ALU.add, reads=[d["psU_r"], d["R_r"]], writes=[d["R_r"]])
                    B.copy(d["Rb"][:, :], d["R"][:, :], reads=[d["R_r"]], writes=[d["Rb_r"]], eng="dve")
                yield

        for pair in range(HR // 2):
            hs = [2 * pair, 2 * pair + 1]
            for j, h in enumerate(hs):
                load(h, hb[j])
            round_robin([chain(h, hb[j]) for j, h in enumerate(hs)])
            for j, h in enumerate(hs):
                d = hb[j]
                nb = (d["psS"], d["psS_r"], d["psO"], d["psO_r"], nbs[0], nbr[0], nbs[1], nbr[1], nbs[2], nbr[2], nbs[3], nbr[3])
                fm_norm(B, d["oall"], d["oall_r"], n_tok, "gn", gw[:, h:h + 1], gw_r, d["gT"], d["gT_r"], scr["CAT"][8 + h], cst, nb)
        s.barrier()
        B.stack = old


def hgrn_stage(B, lb, lb_r, layer, hnw, cst, scr, n_tok=S):
    s = B.s
    NC = n_tok // 64
    with ExitStack() as sub:
        B.stack, old = sub, B.stack
        ident = cst["ident"]
        c_r = cst["c_r"]
        hw = B.sb([128, HH], F32); hw_r = Res()
        B.dma("sp", hw[:, :], hnw.rearrange("(h p) -> p h", p=128), writes=[hw_r], allow_slow_non_contiguous=True)
        q = B.sb([128, n_tok], F32); q_r = Res()
        f = B.sb([128, n_tok], F32); f_r = Res()
        kk = B.sb([128, n_tok], F32); kk_r = Res()
        bb = B.sb([128, n_tok], F32); bb_r = Res()
        e1 = B.sb([128, n_tok], F32); e1_r = Res()
        e2 = B.sb([128, n_tok], F32); e2_r = Res()
        hb = []
        for j in range(2):
            d = {}
            for nm in ("qt", "kt", "qh", "kh", "gT"):
                d[nm] = B.sb([128, n_tok], BF16); d[nm + "_r"] = Res()
            d["ebl"] = B.sb([128, NC], F32); d["ebl_r"] = Res()
            d["vv"] = B.sb([64, NC, 128], BF16); d["vv_r"] = Res()
            d["oall"] = B.sb([128, n_tok], F32); d["oall_r"] = Res()
            d["St"] = B.sb([128, 128], F32); d["St_r"] = Res()
            d["Sb"] = B.sb([128, 128], BF16); d["Sb_r"] = Res()
            d["ATm"] = B.sb([64, 64], BF16); d["ATm_r"] = Res()
            d["khT"] = B.sb([64, 128], BF16); d["khT_r"] = Res()
            d["psS"] = B.ps([128, 512], F32); d["psS_r"] = Res()
            d["psO"] = B.ps([128, 512], F32); d["psO_r"] = Res()
            d["psU"] = B.ps([128, 512], F32); d["psU_r"] = Res()
            d["tp"] = B.ps([128, 8, 128], BF16); d["tp_r"] = Res()
            hb.append(d)
        nbs = e1[:, 0:min(512, n_tok)], e2[:, 0:min(512, n_tok)], bb[:, 0:min(256, n_tok // 2)].bitcast(BF16)
        nbr = e1_r, e2_r, bb_r

        def prep(h, d):
            lbh = lb[:, layer, h:h + 1]
            omh = lb[:, 4 + layer, h:h + 1]
            B.dma("sp", q[:, :], scr["HQ"][h][:, 0:n_tok], writes=[q_r])
            B.dma("sp", f[:, :], scr["HF"][h][:, 0:n_tok], writes=[f_r])
            B.dma("sp", d["gT"][:, :], scr["HG"][h][:, 0:n_tok], writes=[d["gT_r"]])
            B.dma("sp", d["vv"][:, :, :], scr["HI"][0:n_tok, h * 128:(h + 1) * 128].rearrange("(c p) e -> p c e", p=64), writes=[d["vv_r"]])
            B.act(f[:, :], f[:, :], AF.Sigmoid, reads=[f_r], writes=[f_r])
            B.ts(f[:, :], f[:, :], omh, lbh, ALU.mult, ALU.add, reads=[f_r, lb_r], writes=[f_r])
            B.ts(kk[:, :], f[:, :], -1.0, 1.0, ALU.mult, ALU.add, reads=[f_r], writes=[kk_r], eng="pool")
            B.ts(f[:, :], f[:, :], 1e-20, None, ALU.max, reads=[f_r], writes=[f_r])
            B.act(f[:, :], f[:, :], AF.Ln, reads=[f_r], writes=[f_r])
            B.s.op("dve", lambda e, o=bb[:, :], d0=cst["smask"][:, 0:n_tok], d1=f[:, :]: e.tensor_tensor_scan(
                out=o, data0=d0, data1=d1, initial=0.0, op0=ALU.mult, op1=ALU.add), reads=[f_r, c_r], writes=[bb_r])
            b3 = bb[:, :].rearrange("p (c j) -> p c j", j=64)
            bmid = b3[:, :, 31:32].broadcast_to([128, NC, 64])
            blast = b3[:, :, 63:64].broadcast_to([128, NC, 64])
            e13 = e1[:, :].rearrange("p (c j) -> p c j", j=64)
            B.tt(e13, b3, bmid, ALU.subtract, reads=[bb_r], writes=[e1_r])
            B.act(e2[:, :], e1[:, :], AF.Exp, reads=[e1_r], writes=[e2_r])
            B.tt(d["qt"][:, :], q[:, :], e2[:, :], ALU.mult, reads=[q_r, e2_r], writes=[d["qt_r"]])
            B.act(e2[:, :], e1[:, :], AF.Exp, reads=[e1_r], writes=[e2_r], scale=-1.0)
            B.tt(d["kt"][:, :], kk[:, :], e2[:, :], ALU.mult, reads=[kk_r, e2_r], writes=[d["kt_r"]], eng="pool")
            B.act(e2[:, :], bb[:, :], AF.Exp, reads=[bb_r], writes=[e2_r])
            B.tt(d["qh"][:, :], q[:, :], e2[:, :], ALU.mult, reads=[q_r, e2_r], writes=[d["qh_r"]])
            B.copy(d["ebl"][:, :], e2[:, :].rearrange("p (c j) -> p c j", j=64)[:, :, 63], reads=[e2_r], writes=[d["ebl_r"]], eng="dve")
            B.tt(e13, blast, b3, ALU.subtract, reads=[bb_r], writes=[e1_r])
            B.act(e2[:, :], e1[:, :], AF.Exp, reads=[e1_r], writes=[e2_r])
            B.tt(d["kh"][:, :], kk[:, :], e2[:, :], ALU.mult, reads=[kk_r, e2_r], writes=[d["kh_r"]], eng="pool")

        def chain(h, d):
            for c in range(NC):
                cs = slice(c * 64, (c + 1) * 64)
                B.mm(d["psS"][0:64, 0:64], d["kt"][:, cs], d["qt"][:, cs], True, True, reads=[d["kt_r"], d["qt_r"]], writes=[d["psS_r"]])
                if c < NC - 1:
                    B.tr(d["tp"][0:64, 0, :], d["kh"][:, cs], ident[:, :], reads=[d["kh_r"]], writes=[d["tp_r"]])
                yield
                B.tt(d["ATm"][:, :], d["psS"][0:64, 0:64], cst["causT"][:, :], ALU.mult, reads=[d["psS_r"], c_r], writes=[d["ATm_r"]])
                if c < NC - 1:
                    B.copy(d["khT"][:, :], d["tp"][0:64, 0, :], reads=[d["tp_r"]], writes=[d["khT_r"]], eng="act")
                yield
                B.mm(d["psO"][:, 0:64], d["vv"][:, c, :], d["ATm"][:, :], True, c == 0, reads=[d["vv_r"], d["ATm_r"]], writes=[d["psO_r"]])
                if c > 0:
                    B.mm(d["psO"][:, 0:64], d["Sb"][:, :], d["qh"][:, cs], False, True, reads=[d["Sb_r"], d["qh_r"]], writes=[d["psO_r"]])
                if c < NC - 1:
                    B.mm(d["psU"][:, 0:128], d["khT"][:, :], d["vv"][:, c, :], True, True, reads=[d["khT_r"], d["vv_r"]], writes=[d["psU_r"]])
                yield
                B.copy(d["oall"][:, cs], d["psO"][:, 0:64], reads=[d["psO_r"]], writes=[d["oall_r"]], eng="act")
                if c < NC - 1:
                    if c == 0:
                        B.copy(d["St"][:, :], d["psU"][:, 0:128], reads=[d["psU_r"]], writes=[d["St_r"]], eng="dve")
                    else:
                        B.stt(d["St"][:, :], d["St"][:, :], d["ebl"][:, c:c + 1], d["psU"][:, 0:128], ALU.mult, ALU.add,
                              reads=[d["psU_r"], d["St_r"], d["ebl_r"]], writes=[d["St_r"]])
                    B.copy(d["Sb"][:, :], d["St"][:, :], reads=[d["St_r"]], writes=[d["Sb_r"]], eng="dve")
                yield

        for pair in range(HH // 2):
            hs = [2 * pair, 2 * pair + 1]
            for j, h in enumerate(hs):
                prep(h, hb[j])
            round_robin([chain(h, hb[j]) for j, h in enumerate(hs)])
            for j, h in enumerate(hs):
                d = hb[j]
                nb = (d["psS"], d["psS_r"], d["psO"], d["psO_r"], None, Res(), nbs[0], nbr[0], nbs[1], nbr[1], nbs[2], nbr[2])
                fm_norm(B, d["oall"], d["oall_r"], n_tok, "rms", hw[:, h:h + 1], hw_r, d["gT"], d["gT_r"], scr["CAT"][12 + h], cst, nb)
        s.barrier()
        B.stack = old


def load_wo(B, w_o, wo, wo_r):
    wv = w_o.rearrange("(k p) f -> p k f", p=128)
    for k in range(KC):
        B.dma("pool", wo[:, k, :], wv[:, k, :], writes=[wo_r[k]])


def oproj_stage(B, h_dram, h_res, wo, wo_r, scr, n_tok=S):
    s = B.s
    NT = n_tok // 128
    TBk = min(4, NT)
    with ExitStack() as sub:
        B.stack, old = sub, B.stack
        cat = [B.sb([128, KC, TBk * 128], BF16) for _ in range(2)]; cat_r = rl(2, "cat")
        ht = [B.sb([128, D], F32) for _ in range(3)]; ht_r = rl(3, "ht")
        po = [B.ps([128, 512], F32) for _ in range(8)]; po_r = rl(8, "po")
        for c in range(NT):
            cb, cc = c // TBk, c % TBk
            b = cb % 2
            hb_ = c % 3
            if cc == 0:
                B.dma("sp", cat[b][:, :, :], scr["CAT"][:, :, cb * TBk * 128:(cb + 1) * TBk * 128].rearrange("k p t -> p k t"), writes=[cat_r[b]])
            B.dma("sp", ht[hb_][:, :], h_dram[c * 128:(c + 1) * 128, :], reads=[h_res[c]], writes=[ht_r[hb_]])
            for nbk in range(4):
                pi = (c % 2) * 4 + nbk
                for k in range(KC):
                    B.mm(po[pi][:, :], cat[b][:, k, cc * 128:(cc + 1) * 128], wo[:, k, nbk * 512:(nbk + 1) * 512], k == 0, k == KC - 1,
                         reads=[cat_r[b], wo_r[k]], writes=[po_r[pi]])
                hs = ht[hb_][:, nbk * 512:(nbk + 1) * 512]
                B.tt(hs, hs, po[pi][:, :], ALU.add, reads=[po_r[pi], ht_r[hb_]], writes=[ht_r[hb_]])
            B.dma("sp", h_dram[c * 128:(c + 1) * 128, :], ht[hb_][:, :], reads=[ht_r[hb_]], writes=[h_res[c]])
        s.barrier()
        B.stack = old


def final_stage(B, h_dram, h_res, fnw, n_tok=S):
    s = B.s
    NT = n_tok // 128
    with ExitStack() as sub:
        B.stack, old = sub, B.stack
        fw = B.sb([128, D], F32); fw_r = Res()
        B.dma("sp", fw[:, :], fnw.partition_broadcast(128), writes=[fw_r])
        ht = [B.sb([128, D], F32) for _ in range(2)]; ht_r = rl(2, "ht")
        sq = B.sb([128, D], BF16); sq_r = Res()
        ss = [B.sb([128, 4], F32) for _ in range(2)]; ss_r = rl(2, "ss")
        eps = B.sb([128, 1], F32); eps_r = Res()
        B.memset(eps[:, :], EPS, writes=[eps_r])
        for c in range(NT):
            b = c % 2
            B.dma("sp", ht[b][:, :], h_dram[c * 128:(c + 1) * 128, :], reads=[h_res[c]], writes=[ht_r[b]])
            B.act(sq[:, :], ht[b][:, :], AF.Square, reads=[ht_r[b]], writes=[sq_r, ss_r[b]], accum_out=ss[b][:, 0:1])
            B.act(ss[b][:, 1:2], ss[b][:, 0:1], AF.Sqrt, reads=[ss_r[b], eps_r], writes=[ss_r[b]], scale=1.0 / D, bias=eps[:, 0:1])
            B.recip(ss[b][:, 2:3], ss[b][:, 1:2], reads=[ss_r[b]], writes=[ss_r[b]])
            B.stt(ht[b][:, :], ht[b][:, :], ss[b][:, 2:3], fw[:, :], ALU.mult, ALU.mult, reads=[ht_r[b], ss_r[b], fw_r], writes=[ht_r[b]])
            B.dma("sp", h_dram[c * 128:(c + 1) * 128, :], ht[b][:, :], reads=[ht_r[b]], writes=[h_res[c]])
        s.barrier()
        B.stack = old


def lb_compute(B, lg_dram, lb, lb_r):
    with ExitStack() as sub:
        B.stack, old = sub, B.stack
        lg = B.sb([128, 4, 4], F32); r = Res()
        m = B.sb([128, 4], F32)
        B.dma("sp", lg[:, :, :], lg_dram.rearrange("l (h p) -> p l h", p=128), writes=[r], allow_slow_non_contiguous=True)
        B.tt(m[:, :], lg[:, 0, :], lg[:, 1, :], ALU.max, reads=[r], writes=[r])
        B.tt(m[:, :], m[:, :], lg[:, 2, :], ALU.max, reads=[r], writes=[r])
        B.tt(m[:, :], m[:, :], lg[:, 3, :], ALU.max, reads=[r], writes=[r])
        for l in range(4):
            B.tt(lg[:, l, :], lg[:, l, :], m[:, :], ALU.subtract, reads=[r], writes=[r])
        B.act(lg[:, :, :], lg[:, :, :], AF.Exp, reads=[r], writes=[r])
        B.tt(m[:, :], lg[:, 0, :], lg[:, 1, :], ALU.add, reads=[r], writes=[r])
        B.tt(m[:, :], m[:, :], lg[:, 2, :], ALU.add, reads=[r], writes=[r])
        B.tt(m[:, :], m[:, :], lg[:, 3, :], ALU.add, reads=[r], writes=[r])
        B.recip(m[:, :], m[:, :], reads=[r], writes=[r])
        for l in range(4):
            B.tt(lg[:, l, :], lg[:, l, :], m[:, :], ALU.mult, reads=[r], writes=[r])
        B.memset(lb[:, 0, :], 0.0, writes=[lb_r])
        B.copy(lb[:, 1, :], lg[:, 1, :], reads=[r], writes=[lb_r])
        B.tt(lb[:, 2, :], lb[:, 1, :], lg[:, 2, :], ALU.add, reads=[r, lb_r], writes=[lb_r])
        B.tt(lb[:, 3, :], lb[:, 2, :], lg[:, 3, :], ALU.add, reads=[r, lb_r], writes=[lb_r])
        B.ts(lb[:, 4:8, :], lb[:, 0:4, :], -1.0, 1.0, ALU.mult, ALU.add, reads=[lb_r], writes=[lb_r])
        B.s.barrier()
        B.stack = old


def host_consts(n_tok=S):
    import ml_dtypes
    bf = ml_dtypes.bfloat16
    c = {}
    c["ident"] = np.eye(128, dtype=bf)
    qi = np.arange(128)
    c["maskneg"] = np.where(qi[None, :] <= qi[:, None], 0.0, -1e30).astype(bf)
    c["ones"] = np.ones((128, 128), np.float32)

    def rope(dim):
        inv = (10000.0 ** (-np.arange(0, dim, 2, dtype=np.float32) / np.float32(dim))).astype(np.float32)
        ang = (np.arange(n_tok, dtype=np.float32)[:, None] * inv[None, :]).astype(np.float32)
        co, si = np.cos(ang).astype(np.float32).T, np.sin(ang).astype(np.float32).T
        return np.concatenate([co, co], 0), np.concatenate([-si, si], 0)
    c["cosm"], c["sinm"] = rope(64)
    c["cosr"], c["sinr"] = rope(128)
    lg = np.log1p(-(2.0 ** (-5.0 - np.arange(4, dtype=np.float32)))).astype(np.float32)
    idx = np.arange(128, dtype=np.float32)
    rel = idx[None, :] - idx[:, None]
    dm = np.where(rel >= 0, np.exp(lg[:, None, None] * np.maximum(rel, 0.0)[None]), 0.0) * (128.0 ** -0.5)
    c["dmT"] = np.ascontiguousarray(dm.transpose(1, 0, 2)).astype(np.float32)
    xi = np.exp(lg[:, None] * (idx[None, :] + 1.0))
    c["xi"] = np.ascontiguousarray(np.broadcast_to(xi[None], (128, 4, 128))).astype(np.float32)
    zeta = np.exp(lg[:, None] * (127.0 - idx[None, :])) * (128.0 ** -0.5)
    c["zeta"] = np.ascontiguousarray(zeta.T).astype(np.float32)
    j64 = np.arange(64)
    c["causT"] = (j64[None, :] >= j64[:, None]).astype(np.float32)
    sm = np.ones((128, n_tok), np.float32)
    sm[:, ::64] = 0.0
    c["smask"] = sm
    return c


CONST_SPECS = [("ident", [128, 128], BF16), ("maskneg", [128, 128], BF16), ("ones", [128, 128], F32),
               ("dmT", [128, 4, 128], F32), ("xi", [128, 4, 128], F32), ("zeta", [128, 4], F32),
               ("causT", [64, 64], F32)]
TABLE_SPECS = ["cosm", "sinm", "cosr", "sinr", "smask"]
WEIGHT_SPECS = [("ffn1_norm", [DEPTH, D]), ("ffn1_w1", [DEPTH, D, DFF]), ("ffn1_w3", [DEPTH, D, DFF]),
                ("ffn1_w2", [DEPTH, DFF, D]), ("mix_norm", [DEPTH, D]), ("w_in", [DEPTH, D, D_IN]),
                ("mla_kv_norm", [DEPTH, 512]), ("mla_w_kv_b", [DEPTH, 512, 2048]), ("mla_out_norm", [DEPTH, 1024]),
                ("ret_gn", [DEPTH, 512]), ("hgrn_lb_logits", [DEPTH, 512]), ("hgrn_out_norm", [DEPTH, 512]),
                ("w_o", [DEPTH, D, D]), ("ffn2_norm", [DEPTH, D]), ("ffn2_w1", [DEPTH, D, DFF]),
                ("ffn2_w3", [DEPTH, D, DFF]), ("ffn2_w2", [DEPTH, DFF, D]), ("final_norm", [D])]


def build_program(n_tok=S, layers=range(DEPTH), stages=("ffn1", "proj", "mla", "ret", "hgrn", "oproj", "ffn2", "final"),
                  ffn_T=512, dump=None):
    nc = bass.Bass("TRN2", target_bir_lowering=False)
    x = nc.dram_tensor("x", [n_tok, D], F32, kind="ExternalInput").ap()
    W = {name: nc.dram_tensor(name, shp, F32, kind="ExternalInput").ap() for name, shp in WEIGHT_SPECS}
    Cd = {name: nc.dram_tensor(name, shp, dt, kind="ExternalInput").ap() for name, shp, dt in CONST_SPECS}
    for name in TABLE_SPECS:
        rows = 64 if name in ("cosm", "sinm") else 128
        Cd[name] = nc.dram_tensor(name, [rows, n_tok], F32, kind="ExternalInput").ap()
    out = nc.dram_tensor("out", [n_tok, D], F32, kind="ExternalOutput").ap()
    scr = {
        "QN": nc.dram_tensor("s_qn", [HM, 128, n_tok], BF16).ap(), "QR": nc.dram_tensor("s_qr", [HM, 64, n_tok], BF16).ap(),
        "KR": nc.dram_tensor("s_kr", [64, n_tok], BF16).ap(), "CKVN": nc.dram_tensor("s_ckvn", [4, 128, n_tok], BF16).ap(),
        "RQ": nc.dram_tensor("s_rq", [HR, 128, n_tok], BF16).ap(), "RK": nc.dram_tensor("s_rk", [HR, 128, n_tok], BF16).ap(),
        "RV": nc.dram_tensor("s_rv", [n_tok, 512], BF16).ap(), "RG": nc.dram_tensor("s_rg", [HR, 128, n_tok], BF16).ap(),
        "HQ": nc.dram_tensor("s_hq", [HH, 128, n_tok], F32).ap(), "HF": nc.dram_tensor("s_hf", [HH, 128, n_tok], F32).ap(),
        "HI": nc.dram_tensor("s_hi", [n_tok, 512], BF16).ap(), "HG": nc.dram_tensor("s_hg", [HH, 128, n_tok], BF16).ap(),
        "CAT": nc.dram_tensor("s_cat", [16, 128, n_tok], BF16).ap(),
    }
    if dump:
        dump_out = {k: nc.dram_tensor("d_" + k, list(scr[k].shape), scr[k].dtype, kind="ExternalOutput").ap() for k in dump}
    with ExitStack() as stack:
        B = Builder(nc, stack)
        s = B.s
        NT = n_tok // 128
        cst = {"c_r": Res("consts")}
        for name, shp, dt in CONST_SPECS:
            cst[name] = B.sb(shp, dt, "c_" + name)
            B.dma("sp", cst[name][tuple(slice(None) for _ in shp)], Cd[name])
        cst["eps"] = B.sb([128, 1], F32, "c_eps"); cst["eps_r"] = Res("eps")
        B.memset(cst["eps"][:, :], EPS)
        lb = B.sb([128, 8, 4], F32, "c_lb"); lb_r = Res("lb")
        h_res = rl(NT, "h")
        for i in range(NT):
            B.dma("sp", out[i * 128:(i + 1) * 128, :], x[i * 128:(i + 1) * 128, :], writes=[h_res[i]])
        s.barrier()
        lb_compute(B, W["hgrn_lb_logits"], lb, lb_r)

        def run_ffn(l, pre):
            with ExitStack() as sub:
                B.stack, old = sub, B.stack
                st = alloc_ffn_state(B, ffn_T, cst["ident"])
                ffn_stage(B, out, h_res, W[pre + "_norm"][l], W[pre + "_w1"][l], W[pre + "_w3"][l], W[pre + "_w2"][l],
                          ffn_T, st, n_tok=n_tok)
                s.barrier()
                B.stack = old

        for l in layers:
            if "ffn1" in stages:
                run_ffn(l, "ffn1")
            if "proj" in stages:
                with ExitStack() as sub:
                    B.stack, old = sub, B.stack
                    for name in ("cosm", "sinm", "cosr", "sinr"):
                        rows = 64 if name in ("cosm", "sinm") else 128
                        cst[name] = B.sb([rows, n_tok], F32)
                        B.dma("sp", cst[name][:, :], Cd[name])
                    s.barrier()
                    proj_stage(B, out, h_res, W["mix_norm"][l], W["w_in"][l], W["mla_kv_norm"][l], cst, scr, n_tok=n_tok)
                    B.stack = old
            if "mla" in stages:
                mla_stage(B, W["mla_w_kv_b"][l], W["mla_out_norm"][l], cst, scr, n_tok=n_tok)
            if "ret" in stages:
                ret_stage(B, W["ret_gn"][l], cst, scr, n_tok=n_tok)
            with ExitStack() as sub_o:
                B.stack, old_o = sub_o, B.stack
                if "oproj" in stages:
                    wo = B.sb([128, KC, D], BF16); wo_r = rl(KC, "wo")
                    load_wo(B, W["w_o"][l], wo, wo_r)
                if "hgrn" in stages:
                    with ExitStack() as sub:
                        B.stack, old = sub, B.stack
                        cst["smask"] = B.sb([128, n_tok], F32)
                        B.dma("sp", cst["smask"][:, :], Cd["smask"])
                        s.barrier()
                        hgrn_stage(B, lb, lb_r, l, W["hgrn_out_norm"][l], cst, scr, n_tok=n_tok)
                        B.stack = old
                if "oproj" in stages:
                    oproj_stage(B, out, h_res, wo, wo_r, scr, n_tok=n_tok)
                B.stack = old_o
            if "ffn2" in stages:
                run_ffn(l, "ffn2")
        if "final" in stages:
            final_stage(B, out, h_res, W["final_norm"], n_tok=n_tok)
        if dump:
            for k in dump:
                B.dma("sp", dump_out[k], scr[k])
        s.barrier()
        s.wait_all("sp", h_res)
        s.emit(nc, stack)
    return nc


_CONSTS = None


def kernel(**inputs):
    global _CONSTS
    nc = build_program()
    if _CONSTS is None:
        _CONSTS = host_consts(S)
    x = np.ascontiguousarray(inputs["x"], dtype=np.float32)
    shared = {name: np.ascontiguousarray(inputs[name], dtype=np.float32) for name, _ in WEIGHT_SPECS}
    shared.update(_CONSTS)
    in_maps = []
    for c in range(N_CORES):
        m = dict(shared)
        m["x"] = x[c]
        in_maps.append(m)
    res = run_bass_kernel_spmd(nc, in_maps, core_ids=list(range(N_CORES)))
    return np.stack([res.results[c]["out"] for c in range(N_CORES)], axis=0).astype(np.float32)
```
